# Optimizing a Trainium2 kernel written in Bass

```python
import math
import jax
import jax.numpy as jnp
from jax import lax
import numpy as np


D_MODEL = 2048
BATCH = 1
SEQ = 16384
DEPTH = 1

GRID_W = 64
CTX_LEN = 256
EPS = 1e-6

GMLP_GROUPS = 8
GMLP_CH = D_MODEL // 2 // GMLP_GROUPS
D_A = GMLP_GROUPS * GMLP_CH
CHUNK = 128

DIFF_HEADS = 8
DIFF_QK = D_MODEL // 4 // DIFF_HEADS
DIFF_V = 2 * DIFF_QK
D_QK = DIFF_HEADS * 2 * DIFF_QK
D_V = DIFF_HEADS * DIFF_V
ROPE_BASE = 10000.0
Q_BLOCK = 128

D_MIX = D_A + D_V
D_IN = 2 * D_A + 2 * D_QK + D_V

PEER_HEADS = 8
N_KEYS = 128
N_EXPERTS = N_KEYS * N_KEYS
PEER_DK = 256
PEER_TOPK = 16
PEER_BLOCK = 128

kernel_name = 'hybrid_gmlp_diffattn_peer_dit_block'


def rms_norm(x, g):
    xf = x.astype(jnp.float32)
    y = xf * lax.rsqrt(jnp.mean(xf * xf, axis=-1, keepdims=True) + EPS)
    return (y * g.astype(jnp.float32)).astype(x.dtype)


def group_layer_norm(v, g):
    B, L, _ = v.shape
    vf = v.astype(jnp.float32).reshape(B, L, GMLP_GROUPS, GMLP_CH)
    mu = jnp.mean(vf, axis=-1, keepdims=True)
    d = vf - mu
    y = d * lax.rsqrt(jnp.mean(d * d, axis=-1, keepdims=True) + EPS)
    return (y.reshape(B, L, D_A) * g.astype(jnp.float32)).astype(v.dtype)


def modulate(h, shift, scale):
    return h * (1 + scale) + shift


def adaln(cond, w, b):
    mod = jax.nn.silu(cond) @ w + b
    return [m[:, None, :] for m in jnp.split(mod, 6, axis=-1)]


def axial_rope(t):
    n_tok = t.shape[1]
    n_rows = n_tok // GRID_W
    rows = jnp.repeat(jnp.arange(n_rows, dtype=jnp.float32), GRID_W)
    cols = jnp.tile(jnp.arange(GRID_W, dtype=jnp.float32), n_rows)
    n_freq = DIFF_QK // 4
    inv = ROPE_BASE ** (-jnp.arange(n_freq, dtype=jnp.float32) / n_freq)
    ang = jnp.concatenate([rows[:, None] * inv, cols[:, None] * inv], axis=-1)
    cos = jnp.cos(ang)[None, :, None, None, :]
    sin = jnp.sin(ang)[None, :, None, None, :]
    te = t[..., 0::2].astype(jnp.float32)
    to = t[..., 1::2].astype(jnp.float32)
    out = jnp.stack([te * cos - to * sin, te * sin + to * cos], axis=-1)
    return out.reshape(t.shape).astype(t.dtype)


def qk_heads(z, g):
    B, L, _ = z.shape
    return rms_norm(z.reshape(B, L, DIFF_HEADS, 2, DIFF_QK), g)


def mixer_inputs(h, w_in, q_g, k_g, use_rope):
    B, L, _ = h.shape
    z = h @ w_in
    u_a, v_a, q, k, v = jnp.split(z, [D_A, 2 * D_A, 2 * D_A + D_QK, 2 * D_A + 2 * D_QK], axis=-1)
    q = qk_heads(q, q_g)
    k = qk_heads(k, k_g)
    if use_rope:
        q = axial_rope(q)
        k = axial_rope(k)
    return u_a, v_a, q, k, v.reshape(B, L, DIFF_HEADS, DIFF_V)


def context_kv(hc, w_in, k_g):
    B, L, _ = hc.shape
    z = hc @ w_in[:, 2 * D_A + D_QK:]
    k, v = jnp.split(z, [D_QK], axis=-1)
    return qk_heads(k, k_g), v.reshape(B, L, DIFF_HEADS, DIFF_V)


def chunk_gmlp(u, v, ln_g, ws, bs):
    B, L, _ = u.shape
    u = jax.nn.gelu(u, approximate=False)
    v = group_layer_norm(jax.nn.gelu(v, approximate=False), ln_g)
    vc = v.reshape(B, L // CHUNK, CHUNK, GMLP_GROUPS, GMLP_CH)
    mixed = jnp.einsum('gpq,bnqgc->bnpgc', ws, vc) + bs.T[None, None, :, :, None]
    return u * mixed.reshape(B, L, D_A)


def diff_lambda(lq1, lk1, lq2, lk2, lam_init):
    f32 = jnp.float32
    return (jnp.exp(jnp.sum(lq1.astype(f32) * lk1.astype(f32)))
            - jnp.exp(jnp.sum(lq2.astype(f32) * lk2.astype(f32))) + lam_init)


def diff_attention(q, k, v, lam):
    B, Lq = q.shape[0], q.shape[1]
    n_blk = Lq // Q_BLOCK
    qb = jnp.moveaxis(q.reshape(B, n_blk, Q_BLOCK, DIFF_HEADS, 2, DIFF_QK), 1, 0)
    scale = DIFF_QK ** -0.5

    def block(q_blk):
        s = jnp.einsum('bqhcd,bkhcd->bhcqk', q_blk, k).astype(jnp.float32) * scale
        p = jax.nn.softmax(s, axis=-1)
        a = p[:, :, 0] - lam * p[:, :, 1]
        return jnp.einsum('bhqk,bkhd->bqhd', a.astype(v.dtype), v)

    out = lax.map(block, qb)
    return jnp.moveaxis(out, 0, 1).reshape(B, Lq, DIFF_HEADS, DIFF_V)


def merge_mixers(a_out, attn, subln_g, lam_init, w_out):
    B, L = a_out.shape[0], a_out.shape[1]
    attn = rms_norm(attn, subln_g) * (1.0 - lam_init)
    return jnp.concatenate([a_out, attn.reshape(B, L, D_V)], axis=-1) @ w_out


def peer_ffn(h, wq, keys, u_tab, v_tab):
    B, L, D = h.shape
    hb = h.reshape(B * L // PEER_BLOCK, PEER_BLOCK, D)

    def block(ht):
        q = (ht @ wq).reshape(PEER_BLOCK, PEER_HEADS, 2, PEER_DK // 2)
        s = jnp.einsum('thcd,hcnd->thcn', q, keys).astype(jnp.float32)
        s_a, i_a = lax.top_k(s[:, :, 0], PEER_TOPK)
        s_b, i_b = lax.top_k(s[:, :, 1], PEER_TOPK)
        cand_s = (s_a[..., :, None] + s_b[..., None, :]).reshape(PEER_BLOCK, PEER_HEADS, PEER_TOPK * PEER_TOPK)
        cand_i = (i_a[..., :, None] * N_KEYS + i_b[..., None, :]).reshape(PEER_BLOCK, PEER_HEADS, PEER_TOPK * PEER_TOPK)
        top_s, pos = lax.top_k(cand_s, PEER_TOPK)
        idx = jnp.take_along_axis(cand_i, pos, axis=-1)
        gate = jax.nn.softmax(top_s, axis=-1)
        u_e = jnp.take(u_tab, idx, axis=0)
        act = jax.nn.gelu(jnp.einsum('thkd,td->thk', u_e, ht).astype(jnp.float32), approximate=False)
        v_e = jnp.take(v_tab, idx, axis=0)
        return jnp.einsum('thk,thkd->td', (gate * act).astype(v_tab.dtype), v_e)

    return lax.map(block, hb).reshape(B, L, D)


def setup_inputs(seed: int = 0) -> dict:
    key = jax.random.key(seed)
    ks = jax.random.split(key, 26)
    f32 = jnp.float32

    def nrm(k, shape, scale):
        return jax.random.normal(k, shape, f32) * scale

    D = D_MODEL
    return {
        'x': nrm(ks[0], (BATCH, SEQ, D), 1.0),
        'c': nrm(ks[1], (BATCH, D), 1.0),
        'ctx': nrm(ks[2], (BATCH, CTX_LEN, D), 1.0),
        'c_ctx': nrm(ks[3], (D,), 1.0),
        'w_ada': nrm(ks[4], (DEPTH, D, 6 * D), D ** -0.5),
        'b_ada': nrm(ks[5], (DEPTH, 6 * D), 0.02),
        'norm1_g': 1.0 + nrm(ks[6], (DEPTH, D), 0.05),
        'norm2_g': 1.0 + nrm(ks[7], (DEPTH, D), 0.05),
        'w_in': nrm(ks[8], (DEPTH, D, D_IN), D ** -0.5),
        'gmlp_ln_g': 1.0 + nrm(ks[9], (DEPTH, D_A), 0.05),
        'gmlp_ws': nrm(ks[10], (DEPTH, GMLP_GROUPS, CHUNK, CHUNK), 0.5 * CHUNK ** -0.5),
        'gmlp_bs': 1.0 + nrm(ks[11], (DEPTH, GMLP_GROUPS, CHUNK), 0.1),
        'q_norm_g': 1.0 + nrm(ks[12], (DEPTH, DIFF_QK), 0.05),
        'k_norm_g': 1.0 + nrm(ks[13], (DEPTH, DIFF_QK), 0.05),
        'lambda_q1': nrm(ks[14], (DEPTH, DIFF_QK), 0.1),
        'lambda_k1': nrm(ks[15], (DEPTH, DIFF_QK), 0.1),
        'lambda_q2': nrm(ks[16], (DEPTH, DIFF_QK), 0.1),
        'lambda_k2': nrm(ks[17], (DEPTH, DIFF_QK), 0.1),
        'subln_g': 1.0 + nrm(ks[18], (DEPTH, DIFF_V), 0.05),
        'w_out': nrm(ks[19], (DEPTH, D_MIX, D), D_MIX ** -0.5),
        'peer_wq': nrm(ks[20], (DEPTH, D, PEER_HEADS * PEER_DK), D ** -0.5),
        'peer_keys': nrm(ks[21], (DEPTH, PEER_HEADS, 2, N_KEYS, PEER_DK // 2), (PEER_DK // 2) ** -0.5),
        'peer_u': nrm(ks[22], (DEPTH, N_EXPERTS, D), D ** -0.5),
        'peer_v': nrm(ks[23], (DEPTH, N_EXPERTS, D), 0.5),
    }


def reference(x, c, ctx, c_ctx, w_ada, b_ada, norm1_g, norm2_g, w_in, gmlp_ln_g, gmlp_ws, gmlp_bs,
              q_norm_g, k_norm_g, lambda_q1, lambda_k1, lambda_q2, lambda_k2, subln_g, w_out,
              peer_wq, peer_keys, peer_u, peer_v):
    for l in range(DEPTH):
        lam_init = 0.8 - 0.6 * math.exp(-0.3 * l)
        lam = diff_lambda(lambda_q1[l], lambda_k1[l], lambda_q2[l], lambda_k2[l], lam_init)
        sh1, sc1, g1, sh2, sc2, g2 = adaln(c, w_ada[l], b_ada[l])
        mods_c = adaln(c_ctx[None, :], w_ada[l], b_ada[l])

        hc = modulate(rms_norm(ctx, norm1_g[l]), mods_c[0], mods_c[1])
        if l < DEPTH - 1:
            u_c, va_c, q_c, k_c, v_c = mixer_inputs(hc, w_in[l], q_norm_g[l], k_norm_g[l], False)
        else:
            k_c, v_c = context_kv(hc, w_in[l], k_norm_g[l])

        h = modulate(rms_norm(x, norm1_g[l]), sh1, sc1)
        u_a, v_a, q, k, v = mixer_inputs(h, w_in[l], q_norm_g[l], k_norm_g[l], True)
        a_out = chunk_gmlp(u_a, v_a, gmlp_ln_g[l], gmlp_ws[l], gmlp_bs[l])
        attn = diff_attention(q, jnp.concatenate([k_c, k], axis=1), jnp.concatenate([v_c, v], axis=1), lam)
        x_new = x + g1 * merge_mixers(a_out, attn, subln_g[l], lam_init, w_out[l])

        h2 = modulate(rms_norm(x_new, norm2_g[l]), sh2, sc2)
        x_new = x_new + g2 * peer_ffn(h2, peer_wq[l], peer_keys[l], peer_u[l], peer_v[l])

        if l < DEPTH - 1:
            a_c = chunk_gmlp(u_c, va_c, gmlp_ln_g[l], gmlp_ws[l], gmlp_bs[l])
            attn_c = diff_attention(q_c, k_c, v_c, lam)
            ctx = ctx + mods_c[2] * merge_mixers(a_c, attn_c, subln_g[l], lam_init, w_out[l])
            hc2 = modulate(rms_norm(ctx, norm2_g[l]), mods_c[3], mods_c[4])
            ctx = ctx + mods_c[5] * peer_ffn(hc2, peer_wq[l], peer_keys[l], peer_u[l], peer_v[l])
        x = x_new
    return x
```

```python
import numpy as np
import ml_dtypes
import concourse.bass as bass
import concourse.mybir as mybir
from concourse.bass_utils import run_bass_kernel_spmd

F32 = mybir.dt.float32
BF16 = mybir.dt.bfloat16
U8 = mybir.dt.uint8
ALU = mybir.AluOpType
AF = mybir.ActivationFunctionType
AX = mybir.AxisListType

D = 2048
KD = 16
CTX = 256
EPS = 1e-6
LAM_INIT = 0.2
NEXP = 16384
NCORES = 8

ENGS = ("pe", "act", "dve", "pool", "sp")
EPOCH = 30000


class Res:
    __slots__ = ("w", "rs", "excl")

    def __init__(self, excl=False):
        self.w = None
        self.rs = []
        self.excl = excl


class Op:
    __slots__ = ("eng", "emit", "dma", "deps", "needed", "sem", "val", "inc", "slot")

    def __init__(self, eng, emit, dma):
        self.eng = eng
        self.emit = emit
        self.dma = dma
        self.deps = []
        self.needed = False
        self.sem = None
        self.val = 0
        self.inc = 0
        self.slot = None


class Sched:
    def __init__(self, n_dma_slots=8):
        self.ops = {e: [] for e in ENGS}
        self.nslots = n_dma_slots
        self.slot_last = {}
        self.slot_rr = {e: 0 for e in ENGS}
        self.pending_barrier = {e: [] for e in ENGS}

    def op(self, eng, emit, r=(), w=(), dma=False, extra=()):
        o = Op(eng, emit, dma)
        deps = []

        import os
        oldsched = bool(os.environ.get("DBG_OLDSCHED"))
        noself = bool(os.environ.get("DBG_NOSELF"))

        def add(d, kind="raw"):
            if d is None or d is o:
                return
            if (not d.dma) and d.eng == eng and (not dma):
                if eng == "pe" or noself or (oldsched and kind == "war"):
                    return
            deps.append(d)

        for x in r:
            add(x.w)
            if x.excl:
                for rd in x.rs:
                    if rd.eng != eng:
                        add(rd)
        for x in w:
            add(x.w)
            for rd in x.rs:
                add(rd, "war")
        for d in extra:
            add(d)
        for d in self.pending_barrier[eng]:
            add(d)
        self.pending_barrier[eng] = []
        if dma:
            slot = self.slot_rr[eng] % self.nslots
            self.slot_rr[eng] += 1
            o.slot = slot
            prev = self.slot_last.get((eng, slot))
            if prev is not None:
                deps.append(prev)
            self.slot_last[(eng, slot)] = o
        seen = set()
        for d in deps:
            if id(d) in seen:
                continue
            seen.add(id(d))
            d.needed = True
            o.deps.append(d)
        for x in r:
            if dma:
                x.rs.append(o)
            else:
                x.rs = [q for q in x.rs if q.dma or q.eng != eng]
                x.rs.append(o)
        for x in w:
            x.w = o
            x.rs = []
        self.ops[eng].append(o)
        return o

    def barrier(self):
        lasts = []
        for e in ENGS:
            if self.ops[e]:
                lasts.append(self.ops[e][-1])
        for _, o in self.slot_last.items():
            lasts.append(o)
        for e in ENGS:
            self.pending_barrier[e] = list(lasts)

    def finish(self, final_deps):
        self.op("sp", None, extra=final_deps)

    def emit(self, nc):
        sems = {}

        def get_sem(key):
            if key not in sems:
                sems[key] = nc.alloc_semaphore("s_%s" % "_".join(str(k) for k in key))
            return sems[key]

        for e in ENGS:
            cnt = 0
            slot_cnt = {}
            for o in self.ops[e]:
                if o.dma:
                    c = slot_cnt.get(o.slot, 0) + 1
                    slot_cnt[o.slot] = c
                    o.sem = get_sem((e, "d", o.slot))
                    o.val = 16 * c
                    o.inc = 16
                elif o.needed:
                    cnt += 1
                    ep = (cnt - 1) // EPOCH
                    o.sem = get_sem((e, "c", ep))
                    o.val = cnt - ep * EPOCH
                    o.inc = 1
        engmap = {"pe": "tensor", "act": "scalar", "dve": "vector", "pool": "gpsimd", "sp": "sync"}
        with nc.Block() as block:
            for e in ENGS:
                ops = self.ops[e]
                if not ops:
                    continue

                def body(eng, ops=ops):
                    waited = {}
                    for o in ops:
                        need = {}
                        for d in o.deps:
                            key = d.sem.num
                            if d.val > need.get(key, (0, None))[0]:
                                need[key] = (d.val, d.sem)
                        for key, (val, sem) in need.items():
                            if waited.get(key, 0) >= val:
                                continue
                            waited[key] = val
                            eng.wait_ge(sem, val)
                        if o.emit is not None:
                            ins = o.emit(eng)
                            if o.inc:
                                ins.then_inc(o.sem, o.inc)

                getattr(block, engmap[e])(body)


class Arena:
    def __init__(self, tens, size):
        self.t = tens
        self.size = size
        self.off = 0

    def alloc(self, free_shape, dtype):
        n = 1
        for s in free_shape:
            n *= s
        esz = {F32: 4, BF16: 2}[dtype]
        nbytes = (n * esz + 63) // 64 * 64
        assert self.off + nbytes <= self.size, ("SBUF arena overflow", self.off, nbytes, self.size)
        ap = self.t[:, self.off:self.off + n * esz].bitcast(dtype)
        self.off += nbytes
        if len(free_shape) == 2:
            ap = ap.rearrange("p (a b) -> p a b", b=free_shape[1])
        elif len(free_shape) == 3:
            ap = ap.rearrange("p (a b c) -> p a b c", b=free_shape[1], c=free_shape[2])
        return ap

    def mark(self):
        return self.off

    def release(self, m):
        self.off = m


def build_program(SEQ, stage=2):
    TPC = SEQ // NCORES
    NTO = TPC // 128
    NGO = TPC // 512
    NGA = SEQ // 512
    LK = CTX + SEQ
    NKC = LK // 128
    NSEG = 5
    SEGC = (NKC + NSEG - 1) // NSEG

    nc = bass.Bass("TRN2", target_bir_lowering=False)
    import os
    NOPOOL = bool(os.environ.get("DBG_NOPOOL"))

    def din(name, shape, dt=F32):
        return nc.dram_tensor(name, shape, dt, kind="ExternalInput").ap()

    x_all = din("x_all", [SEQ, D])
    x_own = din("x_own", [TPC, D])
    ctx_d = din("ctx", [CTX, D])
    cT_d = din("cT", [128, 32])
    micro = stage in (0.15, 0.16, 0.17)
    if not micro:
        w_ada = din("w_ada", [D, 6 * D])
    bT_d = din("bT", [128, 96])
    bgate_d = din("b_gate", [128, 2 * D])
    n1g_d = din("n1g", [128, 16])
    n2g_d = din("n2g", [128, 16])
    w_in = din("w_in", [D, 5120])
    lng_d = din("lng_bc", [128, 1024])
    wsT_d = din("wsT", [128, 8 * 128])
    bsT_d = din("bsT", [128, 8])
    qkg_d = din("qkg", [128, 4])
    lama_d = din("lam_a", [1, 128])
    lamb_d = din("lam_b", [1, 128])
    gsub_d = din("gsub", [128, 1])
    w_out = din("w_out", [D, D])
    if stage >= 2:
        wq_d = din("wq", [D, D])
        keysT_d = din("keysT", [128, 16 * 128])
        U_d = din("peer_u", [NEXP, D])
        V_d = din("peer_v", [NEXP, D])
    cos_d = din("cosT", [128, SEQ])
    sin_d = din("sinT", [128, SEQ])
    cos_own = din("cos_own", [128, TPC])
    sin_own = din("sin_own", [128, TPC])
    ident_d = din("ident", [128, 128])
    pswap_d = din("pswap", [128, 128])
    bones_d = din("blockones", [128, 128])
    out_d = nc.dram_tensor("out", [TPC, D], F32, kind="ExternalOutput").ap()

    kT_scr = nc.dram_tensor("kT_scr", [8 * 128, LK], BF16, kind="Internal").ap()
    v_scr = nc.dram_tensor("v_scr", [8 * 128, NKC * 128], BF16, kind="Internal").ap()
    xn_scr = nc.dram_tensor("xn_scr", [TPC, D], F32, kind="Internal").ap()

    ARENA = 207 * 1024
    arena_t = nc.alloc_sbuf_tensor("arena", [128, ARENA], U8)
    AR = Arena(arena_t, ARENA)
    ps_all = nc.alloc_psum_tensor("ps_all", [128, 8 * 512], F32)

    def bank(b, n=512, off=0):
        return ps_all[:, b * 512 + off:b * 512 + off + n]

    def bank_bf(b, nbanks=1):
        return ps_all[:, b * 512:(b + nbanks) * 512].bitcast(BF16)

    PB = [Res(excl=True) for _ in range(8)]

    S = Sched()

    def dma(q, out, in_, r=(), w=(), **kw):
        return S.op(q, lambda e: e.dma_start(out=out, in_=in_, **kw), r=r, w=w, dma=True)

    def mm(out, lhsT, rhs, start, stop, r=(), w=()):
        return S.op("pe", lambda e: e.matmul(out, lhsT, rhs, start=start, stop=stop), r=r, w=w)

    def tr(out, in_, ident, r=(), w=()):
        return S.op("pe", lambda e: e.transpose(out, in_, ident), r=r, w=w)

    def act(out, in_, func, r=(), w=(), bias=None, scale=None, accum=None):
        kw = {}
        if bias is not None:
            kw["bias"] = bias
        if scale is not None:
            kw["scale"] = scale
        if accum is not None:
            kw["accum_out"] = accum
        return S.op("act", lambda e: e.activation(out, in_, func, **kw), r=r, w=w)

    def ts(eng, out, in0, s1, s2, op0, op1, r=(), w=()):
        if op1 is None:
            return S.op(eng, lambda e: e.tensor_scalar(out, in0, s1, None, op0), r=r, w=w)
        return S.op(eng, lambda e: e.tensor_scalar(out, in0, s1, s2, op0, op1), r=r, w=w)

    def tt(eng, out, in0, in1, op, r=(), w=()):
        return S.op(eng, lambda e: e.tensor_tensor(out, in0, in1, op), r=r, w=w)

    def stt(out, in0, scalar, in1, op0, op1, r=(), w=()):
        return S.op("dve", lambda e: e.scalar_tensor_tensor(out, in0, scalar, in1, op0, op1), r=r, w=w)

    def cp(eng, out, in_, r=(), w=()):
        if eng == "act":
            return S.op("act", lambda e: e.copy(out, in_), r=r, w=w)
        return S.op(eng, lambda e: e.tensor_copy(out, in_), r=r, w=w)

    def rsqrt_act(out, in_, scale, r_in, w_out, tmp, tmp_res):
        act(tmp, in_, AF.Ln, r=r_in, w=[tmp_res], bias=eps_c[:, 0:1], scale=scale)
        act(out, tmp, AF.Exp, r=[tmp_res], w=w_out, scale=-0.5)

    ident_f = AR.alloc([128], F32)
    pswap_f = AR.alloc([128], F32)
    bones_f = AR.alloc([128], F32)
    ones_f = AR.alloc([128], F32)
    ident_b = AR.alloc([128], BF16)
    eps_c = AR.alloc([1], F32)
    consts = Res()
    dma("sp", ident_f, ident_d, w=[consts])
    dma("sp", pswap_f, pswap_d, w=[consts])
    dma("sp", bones_f, bones_d, w=[consts])
    S.op("dve", lambda e: e.memset(ones_f, 1.0), w=[consts])
    S.op("dve", lambda e: e.memset(eps_c, EPS), w=[consts])
    cp("dve", ident_b, ident_f, r=[consts], w=[consts])
    pswap_b = AR.alloc([128], BF16)
    bones_b = AR.alloc([128], BF16)
    ones_b = AR.alloc([128], BF16)
    cp("dve", pswap_b, pswap_f, r=[consts], w=[consts])
    cp("dve", bones_b, bones_f, r=[consts], w=[consts])
    cp("dve", ones_b, ones_f, r=[consts], w=[consts])

    n1g = AR.alloc([16], F32)
    n2g = AR.alloc([16], F32)
    qkg = AR.alloc([4], F32)
    gsub = AR.alloc([1], F32)
    gsub2 = AR.alloc([1], F32)
    neglam = AR.alloc([1], F32)
    modb = AR.alloc([96, 2], F32)
    A1 = AR.alloc([16], F32)
    A1c = AR.alloc([16], F32)
    A2 = AR.alloc([16], F32)
    g1_bc = AR.alloc([D], F32)
    g2_bc = AR.alloc([D], F32)
    small = Res()
    dma("sp", n1g, n1g_d, w=[small])
    dma("sp", n2g, n2g_d, w=[small])
    dma("sp", qkg, qkg_d, w=[small])
    dma("sp", gsub, gsub_d, w=[small])
    ts("dve", gsub2, gsub, 1.0 - LAM_INIT, None, ALU.mult, None, r=[small], w=[small])

    persist_mark = AR.mark()

    cT = AR.alloc([32], F32)
    sc = AR.alloc([16, 2], F32)
    sc_rep = AR.alloc([16, 128], F32)
    bT = AR.alloc([96], F32)
    bgate = AR.alloc([2 * D], F32)
    wab = [AR.alloc([16, 512], F32) for _ in range(2)]
    wab_r = [Res(), Res()]
    lrow = AR.alloc([256], F32)
    lsm = AR.alloc([8], F32)
    pa = Res()
    dma("sp", cT, cT_d, w=[pa])
    dma("sp", bT, bT_d, w=[pa])
    dma("sp", bgate, bgate_d, w=[pa])
    act(sc.rearrange("p a b -> p (a b)"), cT, AF.Silu, r=[pa], w=[pa])
    cp("dve", sc_rep, sc[:, :, 0:1].to_broadcast([128, 16, 128]), r=[pa], w=[pa])
    dma("sp", lrow[0:1, 0:128], lama_d, w=[pa])
    dma("sp", lrow[0:1, 128:256], lamb_d, w=[pa])
    tt("dve", lrow[0:1, 0:128], lrow[0:1, 0:128], lrow[0:1, 128:256], ALU.mult, r=[pa], w=[pa])
    S.op("dve", lambda e: e.tensor_reduce(lsm[0:1, 0:2], lrow[0:1, 0:128].rearrange("p (a b) -> p a b", b=64),
                                          AX.X, ALU.add), r=[pa], w=[pa])
    act(lsm[0:1, 2:4], lsm[0:1, 0:2], AF.Exp, r=[pa], w=[pa])
    stt(lsm[0:1, 4:5], lsm[0:1, 3:4], -LAM_INIT, lsm[0:1, 2:3], ALU.add, ALU.subtract, r=[pa], w=[pa])
    mm(bank(7, 1), ones_f[0:1, :], lsm[0:1, 4:5], True, True, r=[pa, consts], w=[PB[7]])
    cp("dve", neglam, bank(7, 1), r=[PB[7]], w=[small])
    lsm_dbg = AR.alloc([2], F32)
    cp("dve", lsm_dbg, neglam[:, 0:1].to_broadcast([128, 2]), r=[small], w=[small])

    psm = bank(6, 192)
    if micro:
        S.op("dve", lambda e: e.memset(g1_bc, 0.5), w=[small])
        S.op("dve", lambda e: e.memset(g2_bc, 0.5), w=[small])
        S.op("dve", lambda e: e.memset(wab[0][:, 0, 0:192], 0.001), w=[wab_r[0]])
        mm(psm[:, 0:192], sc_rep[:, 0, :], wab[0][:, 0, 0:192], True, True, r=[pa, wab_r[0]], w=[PB[6]])
    for cb in range(0 if micro else 24):
        buf = wab[cb % 2]
        br = wab_r[cb % 2]
        dma("sp", buf, w_ada[:, cb * 512:(cb + 1) * 512].rearrange("(k p) c -> p k c", p=128), w=[br])
        gate = (8 <= cb < 12) or (20 <= cb < 24)
        if gate:
            pb = 4 + (cb % 2)
            for k in range(KD):
                mm(bank(pb), sc_rep[:, k, :], buf[:, k, :], k == 0, k == KD - 1, r=[pa, br], w=[PB[pb]])
            if cb < 12:
                dst = g1_bc[:, (cb - 8) * 512:(cb - 7) * 512]
                bsrc = bgate[:, (cb - 8) * 512:(cb - 7) * 512]
            else:
                dst = g2_bc[:, (cb - 20) * 512:(cb - 19) * 512]
                bsrc = bgate[:, D + (cb - 20) * 512:D + (cb - 19) * 512]
            tt("dve", dst, bank(pb), bsrc, ALU.add, r=[PB[pb], pa], w=[small])
        else:
            for j in range(4):
                ch = cb * 4 + j
                for k in range(KD):
                    mm(psm[:, ch * 2:ch * 2 + 2], buf[:, k, j * 128:(j + 1) * 128], sc[:, k, :],
                       k == 0, k == KD - 1, r=[pa, br], w=[PB[6]])
    S.op("dve", lambda e: e.memset(modb, 0.0), w=[small])
    for (c0, c1) in ((0, 32), (48, 80)):
        tt("dve", modb[:, c0:c1, :], psm.rearrange("p (a b) -> p a b", b=2)[:, c0:c1, :],
           bT[:, c0:c1].unsqueeze(2).to_broadcast([128, c1 - c0, 2]), ALU.add, r=[PB[6], pa], w=[small])
    stt(A1, modb[:, 16:32, 0], 1.0, n1g, ALU.add, ALU.mult, r=[small], w=[small])
    stt(A1c, modb[:, 16:32, 1], 1.0, n1g, ALU.add, ALU.mult, r=[small], w=[small])
    stt(A2, modb[:, 64:80, 0], 1.0, n2g, ALU.add, ALU.mult, r=[small], w=[small])

    def B1(k):
        return modb[:, k, 0:1]

    def B1c(k):
        return modb[:, k, 1:2]

    def B2(k):
        return modb[:, 48 + k, 0:1]

    def early(items):
        ops = []
        for dst, src, rr in items:
            ops.append(dma("pool", dst, src, r=rr))
        S.finish(ops)
        S.emit(nc)
        return nc

    if stage == 0.1:
        return early([(out_d[0:128, :], g1_bc, [small]), (out_d[128:256, :], g2_bc, [small]),
                      (out_d[256:384, 0:192], modb.rearrange("p a b -> p (a b)"), [small]),
                      (out_d[256:384, 192:208], A1, [small]), (out_d[256:384, 208:224], A1c, [small]),
                      (out_d[256:384, 224:240], A2, [small]), (out_d[256:384, 240:242], lsm_dbg, [small])])
    S.barrier()
    AR.release(persist_mark)

    class HTB:
        def __init__(self, nbuf=2):
            self.nb = nbuf
            self.xt = [AR.alloc([D], F32) for _ in range(nbuf)]
            self.xt_r = [Res() for _ in range(nbuf)]
            self.junk = AR.alloc([D], BF16)
            self.junk_r = Res()
            self.xs = [AR.alloc([D], BF16) for _ in range(nbuf)]
            self.xs_r = [Res() for _ in range(nbuf)]
            self.st = AR.alloc([16], F32)
            self.st_r = [Res() for _ in range(4)]
            self.i = 0

        def tile(self, src, Atab, Bfn, hT, hT_r, col0, pbanks):
            i = self.i
            self.i += 1
            xt, xr = self.xt[i % self.nb], self.xt_r[i % self.nb]
            xs, xsr = self.xs[i % self.nb], self.xs_r[i % self.nb]
            s4 = (i % 4) * 4
            sr = self.st_r[i % 4]
            dma("sp", xt, src, w=[xr])
            act(self.junk, xt, AF.Square, r=[xr], w=[self.junk_r, sr], accum=self.st[:, s4:s4 + 1])
            act(self.st[:, s4 + 1:s4 + 2], self.st[:, s4:s4 + 1], AF.Ln, r=[sr], w=[sr],
                bias=eps_c[:, 0:1], scale=1.0 / D)
            act(self.st[:, s4 + 2:s4 + 3], self.st[:, s4 + 1:s4 + 2], AF.Exp, r=[sr], w=[sr], scale=-0.5)
            ts("dve", xs, xt, self.st[:, s4 + 2:s4 + 3], None, ALU.mult, None, r=[xr, sr], w=[xsr])
            pT = bank_bf(pbanks, 2)
            pr = [PB[pbanks], PB[pbanks + 1]]
            for k in range(KD):
                tr(pT[:, k * 128:(k + 1) * 128], xs[:, k * 128:(k + 1) * 128], ident_b,
                   r=[xsr, consts], w=[pr[k // 8]])
            for k in range(KD):
                o = hT[:, k, col0:col0 + 128]
                src_ps = pT[:, k * 128:(k + 1) * 128]
                if k >= 8:
                    ts("dve", o, src_ps, Atab[:, k:k + 1], Bfn(k), ALU.mult, ALU.add,
                       r=[pr[k // 8], small], w=[hT_r])
                else:
                    act(o, src_ps, AF.Identity, r=[pr[k // 8], small], w=[hT_r],
                        bias=Bfn(k), scale=Atab[:, k:k + 1])

    class QKP:
        def __init__(self):
            self.sq = AR.alloc([512], BF16)
            self.qgb = AR.alloc([512], BF16)
            self.qg = AR.alloc([512], F32)
            self.ln = AR.alloc([512], F32)
            self.rs = AR.alloc([512], F32)
            self.t1 = AR.alloc([512], F32)
            self.t2 = AR.alloc([512], F32)
            self.r = [Res() for _ in range(7)]

        def run(self, psrc, psrc_r, n, gcol, rope, cos, sin, cs_r, out, out_w, b_ss, b_sw):
            sq, qg, ln, rs, t1, t2, qgb = self.sq, self.qg, self.ln, self.rs, self.t1, self.t2, self.qgb
            rq, rg, rl, rr, r1, r2, rgb = self.r
            lvl = int(os.environ.get("DBG_QKP", "99"))
            act(sq[:, :n], psrc, AF.Square, r=[psrc_r], w=[rq])
            if lvl == 1:
                return [rq]
            ts("dve", qg[:, :n], psrc, qkg[:, 2 * gcol:2 * gcol + 1], None, ALU.mult, None, r=[psrc_r, small, rq], w=[rg])
            if lvl == 2:
                return [rq, rg]
            mm(bank(b_ss, n), bones_b, sq[:, :n], True, True, r=[rq, consts], w=[PB[b_ss]])
            if lvl == 3:
                return [rq, rg, PB[b_ss]]
            act(ln[:, :n], bank(b_ss, n), AF.Ln, r=[PB[b_ss]], w=[rl], bias=eps_c[:, 0:1], scale=1.0 / 64)
            if lvl == 4:
                return [rg, rl]
            act(rs[:, :n], ln[:, :n], AF.Exp, r=[rl], w=[rr], scale=-0.5)
            if lvl == 5:
                return [rg, rr]
            if rope:
                act(qgb[:, :n], psrc, AF.Identity, r=[psrc_r, small], w=[rgb], scale=qkg[:, 2 * gcol:2 * gcol + 1])
                mm(bank(b_sw, n), pswap_b, qgb[:, :n], True, True, r=[rgb, consts], w=[PB[b_sw]])
                tt("dve", t1[:, :n], qg[:, :n], cos[:, :n], ALU.mult, r=[rg, cs_r], w=[r1])
                tt("dve", t2[:, :n], bank(b_sw, n), sin[:, :n], ALU.mult, r=[PB[b_sw], cs_r], w=[r2])
                tt("dve" if NOPOOL else "pool", t1[:, :n], t1[:, :n], t2[:, :n], ALU.add, r=[r1, r2], w=[r1])
                tt("dve", out, t1[:, :n], rs[:, :n], ALU.mult, r=[r1, rr], w=out_w)
            else:
                tt("dve", out, qg[:, :n], rs[:, :n], ALU.mult, r=[rg, rr], w=out_w)

    mB = AR.mark()
    wk = AR.alloc([16, 1024], BF16)
    wv = AR.alloc([16, 1024], BF16)
    wkv_r = Res()
    for k in range(KD):
        dma("pool", wk[:, k, :], w_in[k * 128:(k + 1) * 128, 3072:4096], w=[wkv_r])
        dma("pool", wv[:, k, :], w_in[k * 128:(k + 1) * 128, 4096:5120], w=[wkv_r])
    htb = HTB()
    qkp = QKP()
    hT = [AR.alloc([16, 512], BF16) for _ in range(2)]
    hT_r = [Res(), Res()]
    cs = [(AR.alloc([512], F32), AR.alloc([512], F32)) for _ in range(2)]
    cs_r = [Res(), Res()]
    kst = [AR.alloc([512], BF16) for _ in range(2)]
    kst_r = [Res(), Res()]
    vst = [AR.alloc([4, 1024], BF16) for _ in range(2)]
    vst_r = [Res(), Res()]
    scr_w = Res()
    scr_ops = []

    groups = [("ctx", 0, 256)] + [("x", g * 512, 512) for g in range(NGA)]
    kctr = 0
    for gi, (kind, t0, n) in enumerate(groups):
        h_, hr = hT[gi % 2], hT_r[gi % 2]
        ntile = n // 128
        for ti in range(ntile):
            if kind == "ctx":
                src = ctx_d[t0 + ti * 128:t0 + (ti + 1) * 128, :]
                htb.tile(src, A1c, B1c, h_, hr, ti * 128, 0)
            else:
                src = x_all[t0 + ti * 128:t0 + (ti + 1) * 128, :]
                htb.tile(src, A1, B1, h_, hr, ti * 128, 0)
        if stage == 0.16:
            return early([(out_d[0:128, :], g1_bc, [small])])
        key0 = 0 if kind == "ctx" else CTX + t0
        rope = kind != "ctx"
        cos, sin = cs[gi % 2]
        if rope:
            dma("sp", cos, cos_d[:, t0:t0 + 512], w=[cs_r[gi % 2]])
            dma("sp", sin, sin_d[:, t0:t0 + 512], w=[cs_r[gi % 2]])
        for h in range(8):
            pb = 2 + (h % 2)
            for k in range(KD):
                mm(bank(pb, n), wk[:, k, h * 128:(h + 1) * 128], h_[:, k, :n], k == 0, k == KD - 1,
                   r=[wkv_r, hr], w=[PB[pb]])
            ks, ksr = kst[kctr % 2], kst_r[kctr % 2]
            kctr += 1
            if os.environ.get("DBG_SKIP") == "qkp":
                cp("dve", ks[:, :n], bank(pb, n), r=[PB[pb]], w=[ksr])
                return early([(out_d[0:128, 0:128], ks[:, 0:256].bitcast(F32), [ksr])])
            rv = qkp.run(bank(pb, n), PB[pb], n, 1, rope, cos, sin, cs_r[gi % 2], ks[:, :n], [ksr], 4, 5)
            if rv is not None:
                return early([(out_d[0:128, :], g1_bc, [small] + rv)])
            if os.environ.get("DBG_SKIP") != "store":
                scr_ops.append(dma("sp" if NOPOOL else "pool", kT_scr[h * 128:(h + 1) * 128, key0:key0 + n], ks[:, :n], r=[ksr], w=[]))
            if stage == 0.17:
                return early([(out_d[0:128, :], g1_bc, [small])])
        vs, vsr = vst[gi % 2], vst_r[gi % 2]
        for ti in range(ntile):
            for half in range(2):
                pb = 6 + half
                for k in range(KD):
                    mm(bank(pb), h_[:, k, ti * 128:(ti + 1) * 128], wv[:, k, half * 512:(half + 1) * 512],
                       k == 0, k == KD - 1, r=[wkv_r, hr], w=[PB[pb]])
                cp("act", vs[:, ti, half * 512:(half + 1) * 512], bank(pb), r=[PB[pb]], w=[vsr])
        kc0 = key0 // 128
        for h in range(8):
            dst = v_scr[h * 128:(h + 1) * 128, kc0 * 128:(kc0 + ntile) * 128].rearrange("p (t d) -> p t d", d=128)
            scr_ops.append(dma("sp" if NOPOOL else "pool", dst, vs[:, :ntile, h * 128:(h + 1) * 128], r=[vsr], w=[]))
    if stage in (0.2, 0.15):
        return early([(out_d[0:128, :], g1_bc, [small])])
    if stage != 0.206:
        S.barrier()
    if stage == 0.205:
        return early([(out_d[0:128, :], g1_bc, [small])])
    AR.release(mB)

    mergedT = AR.alloc([16, TPC], BF16)
    mg_r = Res()
    mMG = AR.mark()
    QT = AR.alloc([8, TPC], BF16)
    QT_r = Res()
    mB2 = AR.mark()
    wqh = [AR.alloc([16, 128], BF16) for _ in range(2)]
    wq_r = [Res(), Res()]
    htb = HTB()
    qkp = QKP()
    hTq = AR.alloc([16, 512], BF16)
    hTq_r = Res()
    cosq, sinq = AR.alloc([512], F32), AR.alloc([512], F32)
    csq_r = Res()
    wctr2 = 0
    for g in range(NGO):
        for ti in range(4):
            htb.tile(x_own[g * 512 + ti * 128:g * 512 + (ti + 1) * 128, :], A1, B1, hTq, hTq_r, ti * 128, 0)
        if stage in (0.21, 0.206):
            return early([(out_d[0:128, :], g1_bc, [small])])
        dma("sp", cosq, cos_own[:, g * 512:(g + 1) * 512], w=[csq_r])
        dma("sp", sinq, sin_own[:, g * 512:(g + 1) * 512], w=[csq_r])
        for h in range(8):
            if stage == 0.22 and h == 1:
                return early([(out_d[0:128, :], g1_bc, [small])])
            wq_, wqr = wqh[wctr2 % 2], wq_r[wctr2 % 2]
            wctr2 += 1
            import os
            if os.environ.get("DBG_WQ2D"):
                for k in range(KD):
                    dma("pool", wq_[:, k, :], w_in[k * 128:(k + 1) * 128, 2048 + h * 128:2048 + (h + 1) * 128], w=[wqr])
            else:
                dma("pool", wq_, w_in[:, 2048 + h * 128:2048 + (h + 1) * 128].rearrange("(k p) c -> p k c", p=128),
                    w=[wqr])
            pb = 2 + (h % 2)
            for k in range(KD):
                mm(bank(pb), wq_[:, k, :], hTq[:, k, :], k == 0, k == KD - 1, r=[wqr, hTq_r], w=[PB[pb]])
            qkp.run(bank(pb), PB[pb], 512, 0, True, cosq, sinq, csq_r,
                    QT[:, h, g * 512:(g + 1) * 512], [QT_r], 4, 5)
    if stage == 0.3:
        return early([(out_d[0:128, :], g1_bc, [small])])
    S.barrier()
    AR.release(mB2)

    mC = AR.mark()
    KT = AR.alloc([LK], BF16)
    Vs = AR.alloc([NKC, 128], BF16)
    KT_r = [Res() for _ in range(NSEG)]
    V_r = [Res() for _ in range(NSEG)]
    NPT = 6
    pT = [AR.alloc([512], BF16) for _ in range(NPT)]
    pT_r = [Res() for _ in range(NPT)]
    dsb = [AR.alloc([512], BF16) for _ in range(2)]
    rden = [AR.alloc([512], F32) for _ in range(2)]
    o1 = AR.alloc([512], F32)
    o2 = AR.alloc([512], F32)
    at = AR.alloc([512], F32)
    sqb, rsb = dsb[0], o1
    lnb = AR.alloc([512], F32)
    er = [Res() for _ in range(10)]
    pctr = 0
    bctr = 0
    for h in range(8):
        for seg in range(NSEG):
            c0 = seg * SEGC
            c1 = min(NKC, c0 + SEGC)
            if c0 >= c1:
                continue
            dma("sp", KT[:, c0 * 128:c1 * 128], kT_scr[h * 128:(h + 1) * 128, c0 * 128:c1 * 128], w=[KT_r[seg]])
            dma("sp", Vs[:, c0:c1, :],
                v_scr[h * 128:(h + 1) * 128, c0 * 128:c1 * 128].rearrange("p (t d) -> p t d", d=128), w=[V_r[seg]])
        for g in range(NGO):
            qc0 = g * 512

            def pv(kc, cur):
                seg = kc // SEGC
                for comp, p in cur:
                    mm(bank(4 + comp), Vs[:, kc, :], pT[p], kc == 0, kc == NKC - 1,
                       r=[V_r[seg], pT_r[p]], w=[PB[4 + comp]])

            prev = None
            for kc in range(NKC):
                seg = kc // SEGC
                cur = []
                for comp in range(2):
                    b = bctr % 4
                    bctr += 1
                    mm(bank(b), KT[comp * 64:(comp + 1) * 64, kc * 128:(kc + 1) * 128],
                       QT[comp * 64:(comp + 1) * 64, h, qc0:qc0 + 512], True, True,
                       r=[KT_r[seg], QT_r], w=[PB[b]])
                    p = pctr % NPT
                    pctr += 1
                    act(pT[p], bank(b), AF.Exp, r=[PB[b]], w=[pT_r[p]], scale=0.125)
                    if kc == 0:
                        cp("dve", bank(6 + comp), pT[p], r=[pT_r[p]], w=[PB[6 + comp]])
                    else:
                        tt("dve", bank(6 + comp), bank(6 + comp), pT[p], ALU.add,
                           r=[pT_r[p], PB[6 + comp]], w=[PB[6 + comp]])
                    cur.append((comp, p))
                if prev is not None:
                    pv(*prev)
                prev = (kc, cur)
            pv(*prev)
            for comp in range(2):
                cp("act", dsb[comp], bank(6 + comp), r=[PB[6 + comp]], w=[er[comp]])
                mm(bank(comp), ones_b, dsb[comp], True, True, r=[er[comp], consts], w=[PB[comp]])
                S.op("dve", lambda e, c=comp: e.reciprocal(rden[c], bank(c)), r=[PB[comp]], w=[er[2 + comp]])
            tt("dve", o1, bank(4), rden[0], ALU.mult, r=[PB[4], er[2]], w=[er[4]])
            tt("dve", o2, bank(5), rden[1], ALU.mult, r=[PB[5], er[3]], w=[er[5]])
            stt(at, o2, neglam[:, 0:1], o1, ALU.mult, ALU.add, r=[er[4], er[5], small], w=[er[6]])
            act(sqb, at, AF.Square, r=[er[6]], w=[er[0]])
            mm(bank(2), ones_b, sqb, True, True, r=[er[0], consts], w=[PB[2]])
            act(lnb, bank(2), AF.Ln, r=[PB[2]], w=[er[7]], bias=eps_c[:, 0:1], scale=1.0 / 128)
            act(rsb, lnb, AF.Exp, r=[er[7]], w=[er[4]], scale=-0.5)
            stt(mergedT[:, 8 + h, qc0:qc0 + 512], at, gsub2[:, 0:1], rsb, ALU.mult, ALU.mult,
                r=[er[6], er[4], small], w=[mg_r])
    if stage == 0.4:
        return early([(out_d[0:128, :], g1_bc, [small])])
    S.barrier()
    AR.release(mMG)

    mD = AR.mark()
    htb = HTB(1)
    hTd = AR.alloc([16, 512], BF16)
    hTd_r = Res()
    wblk = [AR.alloc([16, 512], BF16) for _ in range(2)]
    wb_r = [Res(), Res()]
    gu = AR.alloc([4, 1024], BF16)
    gv = AR.alloc([4, 1024], F32)
    guv_r = [Res() for _ in range(4)]
    lng = AR.alloc([1024], F32)
    wsT_f = AR.alloc([8, 128], F32)
    wsT_b = AR.alloc([8, 128], BF16)
    bsT = AR.alloc([8], F32)
    tmpA = AR.alloc([1024], F32)
    tmpB = AR.alloc([1024], F32)
    vn = AR.alloc([1024], BF16)
    aout = AR.alloc([1024], BF16)
    st8 = AR.alloc([64], F32)
    dr = [Res() for _ in range(8)]
    gm = Res()
    dma("sp", lng, lng_d, w=[gm])
    dma("sp", wsT_f.rearrange("p a b -> p (a b)"), wsT_d, w=[gm])
    dma("sp", bsT, bsT_d, w=[gm])
    cp("dve", wsT_b, wsT_f, r=[gm], w=[gm])
    wctr = 0
    for g in range(NGO):
        for ti in range(4):
            htb.tile(x_own[g * 512 + ti * 128:g * 512 + (ti + 1) * 128, :], A1, B1, hTd, hTd_r, ti * 128, 0)
        for cb in range(4):
            wb, wbr = wblk[wctr % 2], wb_r[wctr % 2]
            wctr += 1
            dma("pool", wb, w_in[:, cb * 512:(cb + 1) * 512].rearrange("(k p) c -> p k c", p=128), w=[wbr])
            for ti in range(4):
                pb = 4 + ti
                for k in range(KD):
                    mm(bank(pb), hTd[:, k, ti * 128:(ti + 1) * 128], wb[:, k, :], k == 0, k == KD - 1,
                       r=[hTd_r, wbr], w=[PB[pb]])
                if cb < 2:
                    act(gu[:, ti, cb * 512:(cb + 1) * 512], bank(pb), AF.Gelu, r=[PB[pb]], w=[guv_r[ti]])
                else:
                    act(gv[:, ti, (cb - 2) * 512:(cb - 1) * 512], bank(pb), AF.Gelu, r=[PB[pb]], w=[guv_r[ti]])
        for ti in range(4):
            gv2 = gv[:, ti, :]
            gv3 = gv2.rearrange("p (a b) -> p a b", b=128)
            tA3 = tmpA.rearrange("p (a b) -> p a b", b=128)
            tB3 = tmpB.rearrange("p (a b) -> p a b", b=128)
            s1, s2, mean, msq = st8[:, 0:8], st8[:, 8:16], st8[:, 16:24], st8[:, 24:32]
            var, lnv, rstd, nmr = st8[:, 32:40], st8[:, 40:48], st8[:, 48:56], st8[:, 56:64]
            S.op("dve", lambda e, a=s1, b=gv3: e.tensor_reduce(a, b, AX.X, ALU.add), r=[guv_r[ti]], w=[dr[0]])
            act(tmpA, gv2, AF.Square, r=[guv_r[ti]], w=[dr[1]])
            S.op("dve", lambda e, a=s2, b=tA3: e.tensor_reduce(a, b, AX.X, ALU.add), r=[dr[1]], w=[dr[0]])
            ts("dve", mean, s1, 1.0 / 128, None, ALU.mult, None, r=[dr[0]], w=[dr[0]])
            tt("dve", msq, mean, mean, ALU.mult, r=[dr[0]], w=[dr[0]])
            stt(var, s2, 1.0 / 128, msq, ALU.mult, ALU.subtract, r=[dr[0]], w=[dr[0]])
            act(lnv, var, AF.Ln, r=[dr[0]], w=[dr[0]], bias=eps_c[:, 0:1])
            act(rstd, lnv, AF.Exp, r=[dr[0]], w=[dr[0]], scale=-0.5)
            stt(nmr, mean, -1.0, rstd, ALU.mult, ALU.mult, r=[dr[0]], w=[dr[0]])
            tt("dve", tA3, gv3, rstd.unsqueeze(2).to_broadcast([128, 8, 128]), ALU.mult,
               r=[guv_r[ti], dr[0], dr[1]], w=[dr[1]])
            tt("pool", tB3, tA3, nmr.unsqueeze(2).to_broadcast([128, 8, 128]), ALU.add,
               r=[dr[1], dr[0]], w=[dr[2]])
            tt("dve", vn, tmpB, lng, ALU.mult, r=[dr[2], gm], w=[dr[3]])
            for gi in range(8):
                pb = 2 + gi // 4
                mm(bank(pb, 128, (gi % 4) * 128), wsT_b[:, gi, :], vn[:, gi * 128:(gi + 1) * 128], True, True,
                   r=[gm, dr[3]], w=[PB[pb]])
            for gi in range(8):
                pb = 2 + gi // 4
                stt(aout[:, gi * 128:(gi + 1) * 128], bank(pb, 128, (gi % 4) * 128), bsT[:, gi:gi + 1],
                    gu[:, ti, gi * 128:(gi + 1) * 128], ALU.add, ALU.mult, r=[PB[pb], gm, guv_r[ti]], w=[dr[4]])
            pTb = bank_bf(0, 1)
            for gi in range(8):
                tr(pTb[:, gi * 128:(gi + 1) * 128], aout[:, gi * 128:(gi + 1) * 128], ident_b,
                   r=[dr[4], consts], w=[PB[0]])
            tok0 = g * 512 + ti * 128
            cp("act", mergedT[:, 0:8, tok0:tok0 + 128], pTb.rearrange("p (a b) -> p a b", b=128),
               r=[PB[0]], w=[mg_r])
    if stage == 0.5:
        return early([(out_d[0:128, :], g1_bc, [small])])
    S.barrier()
    AR.release(mD)

    mE = AR.mark()
    wo = AR.alloc([16, D], BF16)
    wo_r = Res()
    for k in range(KD):
        dma("pool", wo[:, k, :], w_out[k * 128:(k + 1) * 128, :], w=[wo_r])
    xt2 = [AR.alloc([D], F32) for _ in range(2)]
    xt2_r = [Res(), Res()]
    xnb = [AR.alloc([D], F32) for _ in range(2)]
    xnb_r = [Res(), Res()]
    tmpE = AR.alloc([D], F32)
    tmpE_r = Res()
    out_ops = []
    for ti in range(NTO):
        xt, xr = xt2[ti % 2], xt2_r[ti % 2]
        xo, xor_ = xnb[ti % 2], xnb_r[ti % 2]
        dma("sp", xt, x_own[ti * 128:(ti + 1) * 128, :], w=[xr])
        b0 = 4 * (ti % 2)
        for db in range(4):
            for c in range(16):
                mm(bank(b0 + db), mergedT[:, c, ti * 128:(ti + 1) * 128], wo[:, c, db * 512:(db + 1) * 512],
                   c == 0, c == 15, r=[mg_r, wo_r], w=[PB[b0 + db]])
            tt("dve", tmpE[:, db * 512:(db + 1) * 512], bank(b0 + db), g1_bc[:, db * 512:(db + 1) * 512], ALU.mult,
               r=[PB[b0 + db], small], w=[tmpE_r])
        tt("pool", xo, tmpE, xt, ALU.add, r=[tmpE_r, xr], w=[xor_])
        if stage == 1:
            out_ops.append(dma("pool", out_d[ti * 128:(ti + 1) * 128, :], xo, r=[xor_]))
        else:
            out_ops.append(dma("pool", xn_scr[ti * 128:(ti + 1) * 128, :], xo, r=[xor_]))
    if stage == 1:
        S.finish(out_ops)
        S.emit(nc)
        return nc
    S.barrier()
    AR.release(persist_mark)

    h2T = AR.alloc([16, 512], BF16)
    h2_r = Res()
    htb = HTB(1)
    scs = AR.alloc([4, 16, 128], F32)
    scs_r = [Res() for _ in range(4)]
    thr = AR.alloc([4, 8], F32)
    nbt = AR.alloc([4, 8], F32)
    tn_r = [Res() for _ in range(4)]
    yacc = AR.alloc([4, D], F32)
    y_r = [Res() for _ in range(4)]
    keysb = AR.alloc([16, 128], BF16)
    keys_r = Res()
    dma("pool", keysb.rearrange("p a b -> p (a b)"), keysT_d, w=[keys_r])
    mF = AR.mark()
    fin_ops = []
    NEB = NEXP // 512
    for blk in range(NGO):
        S.barrier()
        AR.release(mF)
        for ti in range(4):
            r0 = blk * 512 + ti * 128
            htb.tile(xn_scr[r0:r0 + 128, :], A2, B2, h2T, h2_r, ti * 128, 0)
        qTs = AR.alloc([16, 512], BF16)
        q_r = Res()
        wqc = [AR.alloc([16, 128], BF16) for _ in range(2)]
        wqc_r = [Res(), Res()]
        for hc in range(16):
            wq_, wqr = wqc[hc % 2], wqc_r[hc % 2]
            dma("pool", wq_, wq_d[:, hc * 128:(hc + 1) * 128].rearrange("(k p) c -> p k c", p=128), w=[wqr])
            pb = 2 + hc % 2
            for k in range(KD):
                mm(bank(pb), wq_[:, k, :], h2T[:, k, :], k == 0, k == KD - 1, r=[wqr, h2_r], w=[PB[pb]])
            cp("act", qTs[:, hc, :], bank(pb), r=[PB[pb]], w=[q_r])
        for ti in range(4):
            for q4 in range(4):
                pb = 4 + q4 % 2
                for j in range(4):
                    hc = q4 * 4 + j
                    mm(bank(pb, 128, j * 128), qTs[:, hc, ti * 128:(ti + 1) * 128], keysb[:, hc, :], True, True,
                       r=[q_r, keys_r], w=[PB[pb]])
                cp("act", scs[:, ti, q4 * 4:(q4 + 1) * 4, :], bank(pb).rearrange("p (a b) -> p a b", b=128),
                   r=[PB[pb]], w=[scs_r[ti]])
        t16 = AR.alloc([16, 16], F32)
        work = AR.alloc([128], F32)
        cand = AR.alloc([8, 256], F32)
        c16 = AR.alloc([8, 16], F32)
        work2 = AR.alloc([256], F32)
        dd = AR.alloc([8, 16], F32)
        zz = AR.alloc([16], F32)
        tk = Res()
        for ti in range(4):
            for hc in range(16):
                S.op("dve", lambda e, a=t16[:, hc, 0:8], b=scs[:, ti, hc, :]: e.max(a, b), r=[scs_r[ti]], w=[tk])
                S.op("dve", lambda e, a=work, b=t16[:, hc, 0:8], c=scs[:, ti, hc, :]: e.match_replace(a, b, c, -1e30),
                     r=[scs_r[ti], tk], w=[tk])
                S.op("dve", lambda e, a=t16[:, hc, 8:16], b=work: e.max(a, b), r=[tk], w=[tk])
            t16v = t16.rearrange("p (h c) k -> p h c k", c=2)
            ta = t16v[:, :, 0, :].unsqueeze(3).to_broadcast([128, 8, 16, 16])
            tb = t16v[:, :, 1, :].unsqueeze(2).to_broadcast([128, 8, 16, 16])
            tt("dve", cand.rearrange("p h (a b) -> p h a b", b=16), ta, tb, ALU.add, r=[tk], w=[tk])
            for h in range(8):
                S.op("dve", lambda e, a=c16[:, h, 0:8], b=cand[:, h, :]: e.max(a, b), r=[tk], w=[tk])
                S.op("dve", lambda e, a=work2, b=c16[:, h, 0:8], c=cand[:, h, :]: e.match_replace(a, b, c, -1e30),
                     r=[tk], w=[tk])
                S.op("dve", lambda e, a=c16[:, h, 8:16], b=work2: e.max(a, b), r=[tk], w=[tk])
            tt("dve", dd, c16, c16[:, :, 0:1].to_broadcast([128, 8, 16]), ALU.subtract, r=[tk], w=[tk])
            act(dd, dd, AF.Exp, r=[tk], w=[tk])
            S.op("dve", lambda e, a=zz[:, 0:8], b=dd: e.tensor_reduce(a, b, AX.X, ALU.add), r=[tk], w=[tk])
            act(zz[:, 8:16], zz[:, 0:8], AF.Ln, r=[tk], w=[tk])
            stt(nbt[:, ti, :], c16[:, :, 0], -1.0, zz[:, 8:16], ALU.mult, ALU.subtract, r=[tk], w=[tn_r[ti]])
            ts("dve", thr[:, ti, :], c16[:, :, 15], -2e-5, None, ALU.add, None, r=[tk], w=[tn_r[ti]])
        S.barrier()
        AR.release(mF)
        Ub = AR.alloc([4, D], BF16)
        Ub_r = Res()
        Vb = [AR.alloc([4, D], BF16)] * 2
        Vb_r = [Res()] * 2
        UT = AR.alloc([16, 512], BF16)
        UT_r = Res()
        gaT = [AR.alloc([512], BF16) for _ in range(4)]
        ga_r = [Res() for _ in range(4)]
        NSB = 3
        Sb = [AR.alloc([512], F32) for _ in range(NSB)]
        Sb_r = [Res() for _ in range(NSB)]
        Eb = [AR.alloc([512], F32) for _ in range(NSB)]
        Eb_r = [Res() for _ in range(NSB)]
        NGH = 4
        Gh = [AR.alloc([512], BF16) for _ in range(NGH)]
        Gh_r = [Res() for _ in range(NGH)]
        PT = [AR.alloc([512], BF16) for _ in range(4)]
        PT_r = [Res() for _ in range(4)]
        sctr = 0
        gctr = 0
        yb = 0
        ectr = 0
        for eb in range(NEB):
            vb, vbr = Vb[eb % 2], Vb_r[eb % 2]
            dma("pool", Ub, U_d[eb * 512:(eb + 1) * 512, :].rearrange("(c p) d -> p c d", p=128), w=[Ub_r])
            dma("pool", vb, V_d[eb * 512:(eb + 1) * 512, :].rearrange("(c p) d -> p c d", p=128), w=[vbr])
            pTb = bank_bf(0, 1)
            for ec in range(4):
                for kg in range(2):
                    for kk in range(8):
                        k = kg * 8 + kk
                        tr(pTb[:, kk * 128:(kk + 1) * 128], Ub[:, ec, k * 128:(k + 1) * 128], ident_b,
                           r=[Ub_r, consts], w=[PB[0]])
                    eng = "act" if ectr % 2 == 0 else "dve"
                    ectr += 1
                    cp(eng, UT[:, kg * 8:(kg + 1) * 8, ec * 128:(ec + 1) * 128],
                       pTb.rearrange("p (a b) -> p a b", b=128), r=[PB[0]], w=[UT_r])
            for ec in range(4):
                for k in range(KD):
                    mm(bank(1), UT[:, k, ec * 128:(ec + 1) * 128], h2T[:, k, :], k == 0, k == KD - 1,
                       r=[UT_r, h2_r], w=[PB[1]])
                act(gaT[ec], bank(1), AF.Gelu, r=[PB[1]], w=[ga_r[ec]])
            for ti in range(4):
                for h in range(8):
                    sb_, sbr = Sb[sctr % NSB], Sb_r[sctr % NSB]
                    eb_, ebr = Eb[sctr % NSB], Eb_r[sctr % NSB]
                    sctr += 1
                    gh, ghr = Gh[gctr % NGH], Gh_r[gctr % NGH]
                    gctr += 1
                    sa = scs[:, ti, 2 * h, eb * 4:(eb + 1) * 4].unsqueeze(2).to_broadcast([128, 4, 128])
                    sbb = scs[:, ti, 2 * h + 1, :].unsqueeze(1).to_broadcast([128, 4, 128])
                    tt("pool", sb_.rearrange("p (a b) -> p a b", b=128), sa, sbb, ALU.add, r=[scs_r[ti]], w=[sbr])
                    act(eb_, sb_, AF.Exp, r=[sbr, tn_r[ti]], w=[ebr], bias=nbt[:, ti, h:h + 1])
                    stt(gh, sb_, thr[:, ti, h:h + 1], eb_, ALU.is_ge, ALU.mult, r=[sbr, ebr, tn_r[ti]], w=[ghr])
                    for ec in range(4):
                        mm(bank(4 + ec, 128, ti * 128), gh[:, ec * 128:(ec + 1) * 128], ident_b, h == 0, h == 7,
                           r=[ghr, consts], w=[PB[4 + ec]])
            for ec in range(4):
                tt("dve", PT[ec], bank(4 + ec), gaT[ec], ALU.mult, r=[PB[4 + ec], ga_r[ec]], w=[PT_r[ec]])
            for ti in range(4):
                for db in range(4):
                    pb = 2 + yb % 2
                    yb += 1
                    for ec in range(4):
                        mm(bank(pb), PT[ec][:, ti * 128:(ti + 1) * 128], vb[:, ec, db * 512:(db + 1) * 512],
                           ec == 0, ec == 3, r=[PT_r[ec], vbr], w=[PB[pb]])
                    ysl = yacc[:, ti, db * 512:(db + 1) * 512]
                    if eb == 0:
                        cp("dve", ysl, bank(pb), r=[PB[pb]], w=[y_r[ti]])
                    else:
                        tt("dve", ysl, bank(pb), ysl, ALU.add, r=[PB[pb], y_r[ti]], w=[y_r[ti]])
        for ti in range(4):
            r0 = blk * 512 + ti * 128
            xt, xr = htb.xt[0], htb.xt_r[0]
            dma("sp", xt, xn_scr[r0:r0 + 128, :], w=[xr])
            tt("dve", yacc[:, ti, :], yacc[:, ti, :], g2_bc, ALU.mult, r=[y_r[ti], small], w=[y_r[ti]])
            tt("pool", yacc[:, ti, :], yacc[:, ti, :], xt, ALU.add, r=[y_r[ti], xr], w=[y_r[ti]])
            fin_ops.append(dma("pool", out_d[r0:r0 + 128, :], yacc[:, ti, :], r=[y_r[ti]]))
    S.finish(fin_ops)
    S.emit(nc)
    return nc


_PROG = {}


def _rope_tables(SEQ):
    n_rows = SEQ // 64
    rows = np.repeat(np.arange(n_rows, dtype=np.float32), 64)
    cols = np.tile(np.arange(64, dtype=np.float32), n_rows)
    inv = (np.float32(10000.0) ** (-np.arange(16, dtype=np.float32) / np.float32(16))).astype(np.float32)
    ang = np.concatenate([rows[:, None] * inv, cols[:, None] * inv], axis=-1).astype(np.float32)
    cos = np.cos(ang).astype(np.float32)
    sin = np.sin(ang).astype(np.float32)
    p = np.arange(128)
    fi = (p % 64) // 2
    sgn = np.where((p % 2) == 0, -1.0, 1.0).astype(np.float32)
    cosT = np.ascontiguousarray(cos[:, fi].T)
    sinT = np.ascontiguousarray((sin[:, fi] * sgn[None, :]).T)
    return cosT, sinT


def kernel(x, c, ctx, c_ctx, w_ada, b_ada, norm1_g, norm2_g, w_in, gmlp_ln_g, gmlp_ws, gmlp_bs,
           q_norm_g, k_norm_g, lambda_q1, lambda_k1, lambda_q2, lambda_k2, subln_g, w_out,
           peer_wq, peer_keys, peer_u, peer_v, _stage=2, _cores=NCORES):
    f = lambda a: np.ascontiguousarray(np.asarray(a, dtype=np.float32))
    x = f(x)
    SEQ = x.shape[1]
    TPC = SEQ // NCORES
    key = (SEQ, _stage)
    if key not in _PROG:
        _PROG[key] = build_program(SEQ, _stage)
    nc = _PROG[key]
    x2 = x[0]
    cc = np.stack([f(c)[0], f(c_ctx)], axis=-1)
    cT = np.ascontiguousarray(cc.reshape(16, 128, 2).transpose(1, 0, 2).reshape(128, 32))
    b = f(b_ada)[0]
    bT = np.ascontiguousarray(b.reshape(96, 128).T)
    b_gate = np.ascontiguousarray(np.tile(np.concatenate([b[2 * D:3 * D], b[5 * D:6 * D]])[None, :], (128, 1)))
    n1g = np.ascontiguousarray(f(norm1_g)[0].reshape(16, 128).T)
    n2g = np.ascontiguousarray(f(norm2_g)[0].reshape(16, 128).T)
    lng_bc = np.ascontiguousarray(np.tile(f(gmlp_ln_g)[0][None, :], (128, 1)))
    wsT = np.ascontiguousarray(f(gmlp_ws)[0].transpose(2, 0, 1).reshape(128, 8 * 128))
    bsT = np.ascontiguousarray(f(gmlp_bs)[0].T)
    _qg, _kg = np.tile(f(q_norm_g)[0], 2), np.tile(f(k_norm_g)[0], 2)
    qkg = np.ascontiguousarray(np.stack([_qg, _qg, _kg, _kg], axis=-1))
    lam_a = np.ascontiguousarray(np.concatenate([f(lambda_q1)[0], f(lambda_q2)[0]])[None, :])
    lam_b = np.ascontiguousarray(np.concatenate([f(lambda_k1)[0], f(lambda_k2)[0]])[None, :])
    gsub = np.ascontiguousarray(f(subln_g)[0][:, None])
    keysT = np.ascontiguousarray(f(peer_keys)[0].transpose(3, 0, 1, 2).reshape(128, 16 * 128))
    cosT, sinT = _rope_tables(SEQ)
    ident = np.eye(128, dtype=np.float32)
    idx = np.arange(128)
    pswap = np.zeros((128, 128), np.float32)
    pswap[idx ^ 1, idx] = 1.0
    blockones = (idx[:, None] // 64 == idx[None, :] // 64).astype(np.float32)
    common = {
        "x_all": x2, "ctx": f(ctx)[0], "cT": cT, "w_ada": f(w_ada)[0], "bT": bT, "b_gate": b_gate,
        "n1g": n1g, "n2g": n2g, "w_in": f(w_in)[0], "lng_bc": lng_bc, "wsT": wsT, "bsT": bsT, "qkg": qkg,
        "lam_a": lam_a, "lam_b": lam_b, "gsub": gsub, "w_out": f(w_out)[0], "wq": f(peer_wq)[0],
        "keysT": keysT, "peer_u": f(peer_u)[0], "peer_v": f(peer_v)[0], "cosT": cosT, "sinT": sinT,
        "ident": ident, "pswap": pswap, "blockones": blockones,
    }
    in_maps = []
    for r in range(NCORES):
        m = dict(common)
        m["x_own"] = np.ascontiguousarray(x2[r * TPC:(r + 1) * TPC])
        m["cos_own"] = np.ascontiguousarray(cosT[:, r * TPC:(r + 1) * TPC])
        m["sin_own"] = np.ascontiguousarray(sinT[:, r * TPC:(r + 1) * TPC])
        in_maps.append(m)
    if _stage < 2:
        for m in in_maps:
            for kk in ("wq", "keysT", "peer_u", "peer_v"):
                m.pop(kk, None)
            if _stage in (0.15, 0.16, 0.17):
                m.pop("w_ada", None)
    in_maps = in_maps[:_cores]
    res = run_bass_kernel_spmd(nc, in_maps, core_ids=list(range(_cores)))
    out = np.concatenate([np.asarray(res.results[r]["out"], dtype=np.float32) for r in range(_cores)], axis=0)
    return out[None, :, :]
```

```python
import numpy as np
import ml_dtypes
import concourse.bass as bass
import concourse.mybir as mybir
from concourse.bass_utils import run_bass_kernel_spmd

F32 = mybir.dt.float32
BF16 = mybir.dt.bfloat16
U8 = mybir.dt.uint8
ALU = mybir.AluOpType
AF = mybir.ActivationFunctionType
AX = mybir.AxisListType

D = 2048
KD = 16
CTX = 256
EPS = 1e-6
LAM_INIT = 0.2
NEXP = 16384
NCORES = 8

ENGS = ("pe", "act", "dve", "pool", "sp")
EPOCH = 30000


class Res:
    __slots__ = ("w", "rs", "excl")

    def __init__(self, excl=False):
        self.w = None
        self.rs = []
        self.excl = excl


class Op:
    __slots__ = ("eng", "emit", "dma", "deps", "needed", "sem", "val", "inc", "slot")

    def __init__(self, eng, emit, dma):
        self.eng = eng
        self.emit = emit
        self.dma = dma
        self.deps = []
        self.needed = False
        self.sem = None
        self.val = 0
        self.inc = 0
        self.slot = None


class Sched:
    def __init__(self, n_dma_slots=8):
        self.ops = {e: [] for e in ENGS}
        self.nslots = n_dma_slots
        self.slot_last = {}
        self.slot_rr = {e: 0 for e in ENGS}
        self.pending_barrier = {e: [] for e in ENGS}

    def op(self, eng, emit, r=(), w=(), dma=False, extra=()):
        o = Op(eng, emit, dma)
        deps = []

        import os
        oldsched = bool(os.environ.get("DBG_OLDSCHED"))
        noself = bool(os.environ.get("DBG_NOSELF"))

        def add(d, kind="raw"):
            if d is None or d is o:
                return
            if (not d.dma) and d.eng == eng and (not dma):
                if eng == "pe" or noself or (oldsched and kind == "war"):
                    return
            deps.append(d)

        for x in r:
            add(x.w)
            if x.excl:
                for rd in x.rs:
                    if rd.eng != eng:
                        add(rd)
        for x in w:
            add(x.w)
            for rd in x.rs:
                add(rd, "war")
        for d in extra:
            add(d)
        for d in self.pending_barrier[eng]:
            add(d)
        self.pending_barrier[eng] = []
        if dma:
            slot = self.slot_rr[eng] % self.nslots
            self.slot_rr[eng] += 1
            o.slot = slot
            prev = self.slot_last.get((eng, slot))
            if prev is not None:
                deps.append(prev)
            self.slot_last[(eng, slot)] = o
        seen = set()
        for d in deps:
            if id(d) in seen:
                continue
            seen.add(id(d))
            d.needed = True
            o.deps.append(d)
        for x in r:
            if dma:
                x.rs.append(o)
            else:
                x.rs = [q for q in x.rs if q.dma or q.eng != eng]
                x.rs.append(o)
        for x in w:
            x.w = o
            x.rs = []
        self.ops[eng].append(o)
        return o

    def barrier(self):
        lasts = []
        for e in ENGS:
            if self.ops[e]:
                lasts.append(self.ops[e][-1])
        for _, o in self.slot_last.items():
            lasts.append(o)
        for e in ENGS:
            self.pending_barrier[e] = list(lasts)

    def finish(self, final_deps):
        self.op("sp", None, extra=final_deps)

    def emit(self, nc):
        sems = {}

        def get_sem(key):
            if key not in sems:
                sems[key] = nc.alloc_semaphore("s_%s" % "_".join(str(k) for k in key))
            return sems[key]

        for e in ENGS:
            cnt = 0
            slot_cnt = {}
            for o in self.ops[e]:
                if o.dma:
                    c = slot_cnt.get(o.slot, 0) + 1
                    slot_cnt[o.slot] = c
                    o.sem = get_sem((e, "d", o.slot))
                    o.val = 16 * c
                    o.inc = 16
                elif o.needed:
                    cnt += 1
                    ep = (cnt - 1) // EPOCH
                    o.sem = get_sem((e, "c", ep))
                    o.val = cnt - ep * EPOCH
                    o.inc = 1
        engmap = {"pe": "tensor", "act": "scalar", "dve": "vector", "pool": "gpsimd", "sp": "sync"}
        with nc.Block() as block:
            for e in ENGS:
                ops = self.ops[e]
                if not ops:
                    continue

                def body(eng, ops=ops):
                    waited = {}
                    for o in ops:
                        need = {}
                        for d in o.deps:
                            key = d.sem.num
                            if d.val > need.get(key, (0, None))[0]:
                                need[key] = (d.val, d.sem)
                        for key, (val, sem) in need.items():
                            if waited.get(key, 0) >= val:
                                continue
                            waited[key] = val
                            eng.wait_ge(sem, val)
                        if o.emit is not None:
                            ins = o.emit(eng)
                            if o.inc:
                                ins.then_inc(o.sem, o.inc)

                getattr(block, engmap[e])(body)


class Arena:
    def __init__(self, tens, size):
        self.t = tens
        self.size = size
        self.off = 0

    def alloc(self, free_shape, dtype):
        n = 1
        for s in free_shape:
            n *= s
        esz = {F32: 4, BF16: 2}[dtype]
        nbytes = (n * esz + 63) // 64 * 64
        assert self.off + nbytes <= self.size, ("SBUF arena overflow", self.off, nbytes, self.size)
        ap = self.t[:, self.off:self.off + n * esz].bitcast(dtype)
        self.off += nbytes
        if len(free_shape) == 2:
            ap = ap.rearrange("p (a b) -> p a b", b=free_shape[1])
        elif len(free_shape) == 3:
            ap = ap.rearrange("p (a b c) -> p a b c", b=free_shape[1], c=free_shape[2])
        return ap

    def mark(self):
        return self.off

    def release(self, m):
        self.off = m


def build_program(SEQ, stage=2):
    TPC = SEQ // NCORES
    NTO = TPC // 128
    NGO = TPC // 512
    NGA = SEQ // 512
    LK = CTX + SEQ
    NKC = LK // 128
    NSEG = 5
    SEGC = (NKC + NSEG - 1) // NSEG

    nc = bass.Bass("TRN2", target_bir_lowering=False)
    import os
    NOPOOL = bool(os.environ.get("DBG_NOPOOL"))

    def din(name, shape, dt=F32):
        return nc.dram_tensor(name, shape, dt, kind="ExternalInput").ap()

    x_all = din("x_all", [SEQ, D])
    x_own = din("x_own", [TPC, D])
    ctx_d = din("ctx", [CTX, D])
    cT_d = din("cT", [128, 32])
    micro = stage in (0.15, 0.16, 0.17)
    if not micro:
        w_ada = din("w_ada", [D, 6 * D])
    bT_d = din("bT", [128, 96])
    bgate_d = din("b_gate", [128, 2 * D])
    n1g_d = din("n1g", [128, 16])
    n2g_d = din("n2g", [128, 16])
    w_in = din("w_in", [D, 5120])
    lng_d = din("lng_bc", [128, 1024])
    wsT_d = din("wsT", [128, 8 * 128])
    bsT_d = din("bsT", [128, 8])
    qkg_d = din("qkg", [128, 4])
    lama_d = din("lam_a", [1, 128])
    lamb_d = din("lam_b", [1, 128])
    gsub_d = din("gsub", [128, 1])
    w_out = din("w_out", [D, D])
    if stage >= 2:
        wq_d = din("wq", [D, D])
        keysT_d = din("keysT", [128, 16 * 128])
        U_d = din("peer_u", [NEXP, D])
        V_d = din("peer_v", [NEXP, D])
    cos_d = din("cosT", [128, SEQ])
    sin_d = din("sinT", [128, SEQ])
    cos_own = din("cos_own", [128, TPC])
    sin_own = din("sin_own", [128, TPC])
    ident_d = din("ident", [128, 128])
    pswap_d = din("pswap", [128, 128])
    bones_d = din("blockones", [128, 128])
    out_d = nc.dram_tensor("out", [TPC, D], F32, kind="ExternalOutput").ap()

    kT_scr = nc.dram_tensor("kT_scr", [8 * 128, LK], BF16, kind="Internal").ap()
    v_scr = nc.dram_tensor("v_scr", [8 * 128, NKC * 128], BF16, kind="Internal").ap()
    xn_scr = nc.dram_tensor("xn_scr", [TPC, D], F32, kind="Internal").ap()

    ARENA = 207 * 1024
    arena_t = nc.alloc_sbuf_tensor("arena", [128, ARENA], U8)
    AR = Arena(arena_t, ARENA)
    ps_all = nc.alloc_psum_tensor("ps_all", [128, 8 * 512], F32)

    def bank(b, n=512, off=0):
        return ps_all[:, b * 512 + off:b * 512 + off + n]

    def bank_bf(b, nbanks=1):
        return ps_all[:, b * 512:(b + nbanks) * 512].bitcast(BF16)

    PB = [Res(excl=True) for _ in range(8)]

    S = Sched()

    def dma(q, out, in_, r=(), w=(), **kw):
        return S.op(q, lambda e: e.dma_start(out=out, in_=in_, **kw), r=r, w=w, dma=True)

    def mm(out, lhsT, rhs, start, stop, r=(), w=()):
        return S.op("pe", lambda e: e.matmul(out, lhsT, rhs, start=start, stop=stop), r=r, w=w)

    def tr(out, in_, ident, r=(), w=()):
        return S.op("pe", lambda e: e.transpose(out, in_, ident), r=r, w=w)

    def act(out, in_, func, r=(), w=(), bias=None, scale=None, accum=None):
        kw = {}
        if bias is not None:
            kw["bias"] = bias
        if scale is not None:
            kw["scale"] = scale
        if accum is not None:
            kw["accum_out"] = accum
        return S.op("act", lambda e: e.activation(out, in_, func, **kw), r=r, w=w)

    def ts(eng, out, in0, s1, s2, op0, op1, r=(), w=()):
        if op1 is None:
            return S.op(eng, lambda e: e.tensor_scalar(out, in0, s1, None, op0), r=r, w=w)
        return S.op(eng, lambda e: e.tensor_scalar(out, in0, s1, s2, op0, op1), r=r, w=w)

    def tt(eng, out, in0, in1, op, r=(), w=()):
        return S.op(eng, lambda e: e.tensor_tensor(out, in0, in1, op), r=r, w=w)

    def stt(out, in0, scalar, in1, op0, op1, r=(), w=()):
        return S.op("dve", lambda e: e.scalar_tensor_tensor(out, in0, scalar, in1, op0, op1), r=r, w=w)

    def cp(eng, out, in_, r=(), w=()):
        if eng == "act":
            return S.op("act", lambda e: e.copy(out, in_), r=r, w=w)
        return S.op(eng, lambda e: e.tensor_copy(out, in_), r=r, w=w)

    def rsqrt_act(out, in_, scale, r_in, w_out, tmp, tmp_res):
        act(tmp, in_, AF.Ln, r=r_in, w=[tmp_res], bias=eps_c[:, 0:1], scale=scale)
        act(out, tmp, AF.Exp, r=[tmp_res], w=w_out, scale=-0.5)

    ident_f = AR.alloc([128], F32)
    pswap_f = AR.alloc([128], F32)
    bones_f = AR.alloc([128], F32)
    ones_f = AR.alloc([128], F32)
    ident_b = AR.alloc([128], BF16)
    eps_c = AR.alloc([1], F32)
    consts = Res()
    dma("sp", ident_f, ident_d, w=[consts])
    dma("sp", pswap_f, pswap_d, w=[consts])
    dma("sp", bones_f, bones_d, w=[consts])
    S.op("dve", lambda e: e.memset(ones_f, 1.0), w=[consts])
    S.op("dve", lambda e: e.memset(eps_c, EPS), w=[consts])
    cp("dve", ident_b, ident_f, r=[consts], w=[consts])
    pswap_b = AR.alloc([128], BF16)
    bones_b = AR.alloc([128], BF16)
    ones_b = AR.alloc([128], BF16)
    cp("dve", pswap_b, pswap_f, r=[consts], w=[consts])
    cp("dve", bones_b, bones_f, r=[consts], w=[consts])
    cp("dve", ones_b, ones_f, r=[consts], w=[consts])

    n1g = AR.alloc([16], F32)
    n2g = AR.alloc([16], F32)
    qkg = AR.alloc([4], F32)
    gsub = AR.alloc([1], F32)
    gsub2 = AR.alloc([1], F32)
    neglam = AR.alloc([1], F32)
    modb = AR.alloc([96, 2], F32)
    A1 = AR.alloc([16], F32)
    A1c = AR.alloc([16], F32)
    A2 = AR.alloc([16], F32)
    g1_bc = AR.alloc([D], F32)
    g2_bc = AR.alloc([D], F32)
    small = Res()
    dma("sp", n1g, n1g_d, w=[small])
    dma("sp", n2g, n2g_d, w=[small])
    dma("sp", qkg, qkg_d, w=[small])
    dma("sp", gsub, gsub_d, w=[small])
    ts("dve", gsub2, gsub, 1.0 - LAM_INIT, None, ALU.mult, None, r=[small], w=[small])

    persist_mark = AR.mark()

    cT = AR.alloc([32], F32)
    sc = AR.alloc([16, 2], F32)
    sc_rep = AR.alloc([16, 128], F32)
    bT = AR.alloc([96], F32)
    bgate = AR.alloc([2 * D], F32)
    wab = [AR.alloc([16, 512], F32) for _ in range(2)]
    wab_r = [Res(), Res()]
    lrow = AR.alloc([256], F32)
    lsm = AR.alloc([8], F32)
    pa = Res()
    dma("sp", cT, cT_d, w=[pa])
    dma("sp", bT, bT_d, w=[pa])
    dma("sp", bgate, bgate_d, w=[pa])
    act(sc.rearrange("p a b -> p (a b)"), cT, AF.Silu, r=[pa], w=[pa])
    cp("dve", sc_rep, sc[:, :, 0:1].to_broadcast([128, 16, 128]), r=[pa], w=[pa])
    dma("sp", lrow[0:1, 0:128], lama_d, w=[pa])
    dma("sp", lrow[0:1, 128:256], lamb_d, w=[pa])
    tt("dve", lrow[0:1, 0:128], lrow[0:1, 0:128], lrow[0:1, 128:256], ALU.mult, r=[pa], w=[pa])
    S.op("dve", lambda e: e.tensor_reduce(lsm[0:1, 0:2], lrow[0:1, 0:128].rearrange("p (a b) -> p a b", b=64),
                                          AX.X, ALU.add), r=[pa], w=[pa])
    act(lsm[0:1, 2:4], lsm[0:1, 0:2], AF.Exp, r=[pa], w=[pa])
    stt(lsm[0:1, 4:5], lsm[0:1, 3:4], -LAM_INIT, lsm[0:1, 2:3], ALU.add, ALU.subtract, r=[pa], w=[pa])
    mm(bank(7, 1), ones_f[0:1, :], lsm[0:1, 4:5], True, True, r=[pa, consts], w=[PB[7]])
    cp("dve", neglam, bank(7, 1), r=[PB[7]], w=[small])
    lsm_dbg = AR.alloc([2], F32)
    cp("dve", lsm_dbg, neglam[:, 0:1].to_broadcast([128, 2]), r=[small], w=[small])

    psm = bank(6, 192)
    if micro:
        S.op("dve", lambda e: e.memset(g1_bc, 0.5), w=[small])
        S.op("dve", lambda e: e.memset(g2_bc, 0.5), w=[small])
        S.op("dve", lambda e: e.memset(wab[0][:, 0, 0:192], 0.001), w=[wab_r[0]])
        mm(psm[:, 0:192], sc_rep[:, 0, :], wab[0][:, 0, 0:192], True, True, r=[pa, wab_r[0]], w=[PB[6]])
    for cb in range(0 if micro else 24):
        buf = wab[cb % 2]
        br = wab_r[cb % 2]
        dma("sp", buf, w_ada[:, cb * 512:(cb + 1) * 512].rearrange("(k p) c -> p k c", p=128), w=[br])
        gate = (8 <= cb < 12) or (20 <= cb < 24)
        if gate:
            pb = 4 + (cb % 2)
            for k in range(KD):
                mm(bank(pb), sc_rep[:, k, :], buf[:, k, :], k == 0, k == KD - 1, r=[pa, br], w=[PB[pb]])
            if cb < 12:
                dst = g1_bc[:, (cb - 8) * 512:(cb - 7) * 512]
                bsrc = bgate[:, (cb - 8) * 512:(cb - 7) * 512]
            else:
                dst = g2_bc[:, (cb - 20) * 512:(cb - 19) * 512]
                bsrc = bgate[:, D + (cb - 20) * 512:D + (cb - 19) * 512]
            tt("dve", dst, bank(pb), bsrc, ALU.add, r=[PB[pb], pa], w=[small])
        else:
            for j in range(4):
                ch = cb * 4 + j
                for k in range(KD):
                    mm(psm[:, ch * 2:ch * 2 + 2], buf[:, k, j * 128:(j + 1) * 128], sc[:, k, :],
                       k == 0, k == KD - 1, r=[pa, br], w=[PB[6]])
    S.op("dve", lambda e: e.memset(modb, 0.0), w=[small])
    for (c0, c1) in ((0, 32), (48, 80)):
        tt("dve", modb[:, c0:c1, :], psm.rearrange("p (a b) -> p a b", b=2)[:, c0:c1, :],
           bT[:, c0:c1].unsqueeze(2).to_broadcast([128, c1 - c0, 2]), ALU.add, r=[PB[6], pa], w=[small])
    stt(A1, modb[:, 16:32, 0], 1.0, n1g, ALU.add, ALU.mult, r=[small], w=[small])
    stt(A1c, modb[:, 16:32, 1], 1.0, n1g, ALU.add, ALU.mult, r=[small], w=[small])
    stt(A2, modb[:, 64:80, 0], 1.0, n2g, ALU.add, ALU.mult, r=[small], w=[small])

    def B1(k):
        return modb[:, k, 0:1]

    def B1c(k):
        return modb[:, k, 1:2]

    def B2(k):
        return modb[:, 48 + k, 0:1]

    def early(items):
        ops = []
        for dst, src, rr in items:
            ops.append(dma("pool", dst, src, r=rr))
        S.finish(ops)
        S.emit(nc)
        return nc

    if stage == 0.1:
        return early([(out_d[0:128, :], g1_bc, [small]), (out_d[128:256, :], g2_bc, [small]),
                      (out_d[256:384, 0:192], modb.rearrange("p a b -> p (a b)"), [small]),
                      (out_d[256:384, 192:208], A1, [small]), (out_d[256:384, 208:224], A1c, [small]),
                      (out_d[256:384, 224:240], A2, [small]), (out_d[256:384, 240:242], lsm_dbg, [small])])
    S.barrier()
    AR.release(persist_mark)

    class HTB:
        def __init__(self, nbuf=2):
            self.nb = nbuf
            self.xt = [AR.alloc([D], F32) for _ in range(nbuf)]
            self.xt_r = [Res() for _ in range(nbuf)]
            self.junk = AR.alloc([D], BF16)
            self.junk_r = Res()
            self.xs = [AR.alloc([D], BF16) for _ in range(nbuf)]
            self.xs_r = [Res() for _ in range(nbuf)]
            self.st = AR.alloc([16], F32)
            self.st_r = [Res() for _ in range(4)]
            self.i = 0

        def tile(self, src, Atab, Bfn, hT, hT_r, col0, pbanks):
            i = self.i
            self.i += 1
            xt, xr = self.xt[i % self.nb], self.xt_r[i % self.nb]
            xs, xsr = self.xs[i % self.nb], self.xs_r[i % self.nb]
            s4 = (i % 4) * 4
            sr = self.st_r[i % 4]
            dma("sp", xt, src, w=[xr])
            act(self.junk, xt, AF.Square, r=[xr], w=[self.junk_r, sr], accum=self.st[:, s4:s4 + 1])
            act(self.st[:, s4 + 1:s4 + 2], self.st[:, s4:s4 + 1], AF.Ln, r=[sr], w=[sr],
                bias=eps_c[:, 0:1], scale=1.0 / D)
            act(self.st[:, s4 + 2:s4 + 3], self.st[:, s4 + 1:s4 + 2], AF.Exp, r=[sr], w=[sr], scale=-0.5)
            ts("dve", xs, xt, self.st[:, s4 + 2:s4 + 3], None, ALU.mult, None, r=[xr, sr], w=[xsr])
            pT = bank_bf(pbanks, 2)
            pr = [PB[pbanks], PB[pbanks + 1]]
            for k in range(KD):
                tr(pT[:, k * 128:(k + 1) * 128], xs[:, k * 128:(k + 1) * 128], ident_b,
                   r=[xsr, consts], w=[pr[k // 8]])
            for k in range(KD):
                o = hT[:, k, col0:col0 + 128]
                src_ps = pT[:, k * 128:(k + 1) * 128]
                if k >= 8:
                    ts("dve", o, src_ps, Atab[:, k:k + 1], Bfn(k), ALU.mult, ALU.add,
                       r=[pr[k // 8], small], w=[hT_r])
                else:
                    act(o, src_ps, AF.Identity, r=[pr[k // 8], small], w=[hT_r],
                        bias=Bfn(k), scale=Atab[:, k:k + 1])

    class QKP:
        def __init__(self):
            self.sq = AR.alloc([512], BF16)
            self.qgb = AR.alloc([512], BF16)
            self.qg = AR.alloc([512], F32)
            self.ln = AR.alloc([512], F32)
            self.rs = AR.alloc([512], F32)
            self.t1 = AR.alloc([512], F32)
            self.t2 = AR.alloc([512], F32)
            self.r = [Res() for _ in range(7)]

        def run(self, psrc, psrc_r, n, gcol, rope, cos, sin, cs_r, out, out_w, b_ss, b_sw):
            sq, qg, ln, rs, t1, t2, qgb = self.sq, self.qg, self.ln, self.rs, self.t1, self.t2, self.qgb
            rq, rg, rl, rr, r1, r2, rgb = self.r
            lvl = int(os.environ.get("DBG_QKP", "99"))
            act(sq[:, :n], psrc, AF.Square, r=[psrc_r], w=[rq])
            if lvl == 1:
                return [rq]
            ts("dve", qg[:, :n], psrc, qkg[:, 2 * gcol:2 * gcol + 1], None, ALU.mult, None, r=[psrc_r, small, rq], w=[rg])
            if lvl == 2:
                return [rq, rg]
            mm(bank(b_ss, n), bones_b, sq[:, :n], True, True, r=[rq, consts], w=[PB[b_ss]])
            if lvl == 3:
                return [rq, rg, PB[b_ss]]
            act(ln[:, :n], bank(b_ss, n), AF.Ln, r=[PB[b_ss]], w=[rl], bias=eps_c[:, 0:1], scale=1.0 / 64)
            if lvl == 4:
                return [rg, rl]
            act(rs[:, :n], ln[:, :n], AF.Exp, r=[rl], w=[rr], scale=-0.5)
            if lvl == 5:
                return [rg, rr]
            if rope:
                act(qgb[:, :n], psrc, AF.Identity, r=[psrc_r, small], w=[rgb], scale=qkg[:, 2 * gcol:2 * gcol + 1])
                mm(bank(b_sw, n), pswap_b, qgb[:, :n], True, True, r=[rgb, consts], w=[PB[b_sw]])
                tt("dve", t1[:, :n], qg[:, :n], cos[:, :n], ALU.mult, r=[rg, cs_r], w=[r1])
                tt("dve", t2[:, :n], bank(b_sw, n), sin[:, :n], ALU.mult, r=[PB[b_sw], cs_r], w=[r2])
                tt("dve" if NOPOOL else "pool", t1[:, :n], t1[:, :n], t2[:, :n], ALU.add, r=[r1, r2], w=[r1])
                tt("dve", out, t1[:, :n], rs[:, :n], ALU.mult, r=[r1, rr], w=out_w)
            else:
                tt("dve", out, qg[:, :n], rs[:, :n], ALU.mult, r=[rg, rr], w=out_w)

    mB = AR.mark()
    wk = AR.alloc([16, 1024], BF16)
    wv = AR.alloc([16, 1024], BF16)
    wkv_r = Res()
    for k in range(KD):
        dma("pool", wk[:, k, :], w_in[k * 128:(k + 1) * 128, 3072:4096], w=[wkv_r])
        dma("pool", wv[:, k, :], w_in[k * 128:(k + 1) * 128, 4096:5120], w=[wkv_r])
    htb = HTB()
    qkp = QKP()
    hT = [AR.alloc([16, 512], BF16) for _ in range(2)]
    hT_r = [Res(), Res()]
    cs = [(AR.alloc([512], F32), AR.alloc([512], F32)) for _ in range(2)]
    cs_r = [Res(), Res()]
    kst = [AR.alloc([512], BF16) for _ in range(2)]
    kst_r = [Res(), Res()]
    vst = [AR.alloc([4, 1024], BF16) for _ in range(2)]
    vst_r = [Res(), Res()]
    scr_w = Res()
    scr_ops = []

    groups = [("ctx", 0, 256)] + [("x", g * 512, 512) for g in range(NGA)]
    kctr = 0
    for gi, (kind, t0, n) in enumerate(groups):
        h_, hr = hT[gi % 2], hT_r[gi % 2]
        ntile = n // 128
        for ti in range(ntile):
            if kind == "ctx":
                src = ctx_d[t0 + ti * 128:t0 + (ti + 1) * 128, :]
                htb.tile(src, A1c, B1c, h_, hr, ti * 128, 0)
            else:
                src = x_all[t0 + ti * 128:t0 + (ti + 1) * 128, :]
                htb.tile(src, A1, B1, h_, hr, ti * 128, 0)
        if stage == 0.16:
            return early([(out_d[0:128, :], g1_bc, [small])])
        key0 = 0 if kind == "ctx" else CTX + t0
        rope = kind != "ctx"
        cos, sin = cs[gi % 2]
        if rope:
            dma("sp", cos, cos_d[:, t0:t0 + 512], w=[cs_r[gi % 2]])
            dma("sp", sin, sin_d[:, t0:t0 + 512], w=[cs_r[gi % 2]])
        for h in range(8):
            pb = 2 + (h % 2)
            for k in range(KD):
                mm(bank(pb, n), wk[:, k, h * 128:(h + 1) * 128], h_[:, k, :n], k == 0, k == KD - 1,
                   r=[wkv_r, hr], w=[PB[pb]])
            ks, ksr = kst[kctr % 2], kst_r[kctr % 2]
            kctr += 1
            if os.environ.get("DBG_SKIP") == "qkp":
                cp("dve", ks[:, :n], bank(pb, n), r=[PB[pb]], w=[ksr])
                return early([(out_d[0:128, 0:128], ks[:, 0:256].bitcast(F32), [ksr])])
            rv = qkp.run(bank(pb, n), PB[pb], n, 1, rope, cos, sin, cs_r[gi % 2], ks[:, :n], [ksr], 4, 5)
            if rv is not None:
                return early([(out_d[0:128, :], g1_bc, [small] + rv)])
            if os.environ.get("DBG_SKIP") != "store":
                scr_ops.append(dma("sp" if NOPOOL else "pool", kT_scr[h * 128:(h + 1) * 128, key0:key0 + n], ks[:, :n], r=[ksr], w=[]))
            if stage == 0.17:
                return early([(out_d[0:128, :], g1_bc, [small])])
        vs, vsr = vst[gi % 2], vst_r[gi % 2]
        for ti in range(ntile):
            for half in range(2):
                pb = 6 + half
                for k in range(KD):
                    mm(bank(pb), h_[:, k, ti * 128:(ti + 1) * 128], wv[:, k, half * 512:(half + 1) * 512],
                       k == 0, k == KD - 1, r=[wkv_r, hr], w=[PB[pb]])
                cp("act", vs[:, ti, half * 512:(half + 1) * 512], bank(pb), r=[PB[pb]], w=[vsr])
        kc0 = key0 // 128
        for h in range(8):
            dst = v_scr[h * 128:(h + 1) * 128, kc0 * 128:(kc0 + ntile) * 128].rearrange("p (t d) -> p t d", d=128)
            scr_ops.append(dma("sp" if NOPOOL else "pool", dst, vs[:, :ntile, h * 128:(h + 1) * 128], r=[vsr], w=[]))
    if stage in (0.2, 0.15):
        return early([(out_d[0:128, :], g1_bc, [small])])
    if stage != 0.206:
        S.barrier()
    if stage == 0.205:
        return early([(out_d[0:128, :], g1_bc, [small])])
    AR.release(mB)

    mergedT = AR.alloc([16, TPC], BF16)
    mg_r = Res()
    mMG = AR.mark()
    QT = AR.alloc([8, TPC], BF16)
    QT_r = Res()
    mB2 = AR.mark()
    wqh = [AR.alloc([16, 128], BF16) for _ in range(2)]
    wq_r = [Res(), Res()]
    htb = HTB()
    qkp = QKP()
    hTq = AR.alloc([16, 512], BF16)
    hTq_r = Res()
    cosq, sinq = AR.alloc([512], F32), AR.alloc([512], F32)
    csq_r = Res()
    wctr2 = 0
    for g in range(NGO):
        for ti in range(4):
            htb.tile(x_own[g * 512 + ti * 128:g * 512 + (ti + 1) * 128, :], A1, B1, hTq, hTq_r, ti * 128, 0)
        if stage in (0.21, 0.206):
            return early([(out_d[0:128, :], g1_bc, [small])])
        dma("sp", cosq, cos_own[:, g * 512:(g + 1) * 512], w=[csq_r])
        dma("sp", sinq, sin_own[:, g * 512:(g + 1) * 512], w=[csq_r])
        for h in range(8):
            if stage == 0.22 and h == 1:
                return early([(out_d[0:128, :], g1_bc, [small])])
            wq_, wqr = wqh[wctr2 % 2], wq_r[wctr2 % 2]
            wctr2 += 1
            import os
            if os.environ.get("DBG_WQ2D"):
                for k in range(KD):
                    dma("pool", wq_[:, k, :], w_in[k * 128:(k + 1) * 128, 2048 + h * 128:2048 + (h + 1) * 128], w=[wqr])
            else:
                dma("pool", wq_, w_in[:, 2048 + h * 128:2048 + (h + 1) * 128].rearrange("(k p) c -> p k c", p=128),
                    w=[wqr])
            pb = 2 + (h % 2)
            for k in range(KD):
                mm(bank(pb), wq_[:, k, :], hTq[:, k, :], k == 0, k == KD - 1, r=[wqr, hTq_r], w=[PB[pb]])
            qkp.run(bank(pb), PB[pb], 512, 0, True, cosq, sinq, csq_r,
                    QT[:, h, g * 512:(g + 1) * 512], [QT_r], 4, 5)
    if stage == 0.3:
        return early([(out_d[0:128, :], g1_bc, [small])])
    S.barrier()
    AR.release(mB2)

    mC = AR.mark()
    KT = AR.alloc([LK], BF16)
    Vs = AR.alloc([NKC, 128], BF16)
    KT_r = [Res() for _ in range(NSEG)]
    V_r = [Res() for _ in range(NSEG)]
    NPT = 6
    pT = [AR.alloc([512], BF16) for _ in range(NPT)]
    pT_r = [Res() for _ in range(NPT)]
    dsb = [AR.alloc([512], BF16) for _ in range(2)]
    rden = [AR.alloc([512], F32) for _ in range(2)]
    o1 = AR.alloc([512], F32)
    o2 = AR.alloc([512], F32)
    at = AR.alloc([512], F32)
    sqb, rsb = dsb[0], o1
    lnb = AR.alloc([512], F32)
    er = [Res() for _ in range(10)]
    pctr = 0
    bctr = 0
    for h in range(8):
        for seg in range(NSEG):
            c0 = seg * SEGC
            c1 = min(NKC, c0 + SEGC)
            if c0 >= c1:
                continue
            dma("sp", KT[:, c0 * 128:c1 * 128], kT_scr[h * 128:(h + 1) * 128, c0 * 128:c1 * 128], w=[KT_r[seg]])
            dma("sp", Vs[:, c0:c1, :],
                v_scr[h * 128:(h + 1) * 128, c0 * 128:c1 * 128].rearrange("p (t d) -> p t d", d=128), w=[V_r[seg]])
        for g in range(NGO):
            qc0 = g * 512

            def pv(kc, cur):
                seg = kc // SEGC
                for comp, p in cur:
                    mm(bank(4 + comp), Vs[:, kc, :], pT[p], kc == 0, kc == NKC - 1,
                       r=[V_r[seg], pT_r[p]], w=[PB[4 + comp]])

            prev = None
            for kc in range(NKC):
                seg = kc // SEGC
                cur = []
                for comp in range(2):
                    b = bctr % 4
                    bctr += 1
                    mm(bank(b), KT[comp * 64:(comp + 1) * 64, kc * 128:(kc + 1) * 128],
                       QT[comp * 64:(comp + 1) * 64, h, qc0:qc0 + 512], True, True,
                       r=[KT_r[seg], QT_r], w=[PB[b]])
                    p = pctr % NPT
                    pctr += 1
                    act(pT[p], bank(b), AF.Exp, r=[PB[b]], w=[pT_r[p]], scale=0.125)
                    if kc == 0:
                        cp("dve", bank(6 + comp), pT[p], r=[pT_r[p]], w=[PB[6 + comp]])
                    else:
                        tt("dve", bank(6 + comp), bank(6 + comp), pT[p], ALU.add,
                           r=[pT_r[p], PB[6 + comp]], w=[PB[6 + comp]])
                    cur.append((comp, p))
                if prev is not None:
                    pv(*prev)
                prev = (kc, cur)
            pv(*prev)
            for comp in range(2):
                cp("act", dsb[comp], bank(6 + comp), r=[PB[6 + comp]], w=[er[comp]])
                mm(bank(comp), ones_b, dsb[comp], True, True, r=[er[comp], consts], w=[PB[comp]])
                S.op("dve", lambda e, c=comp: e.reciprocal(rden[c], bank(c)), r=[PB[comp]], w=[er[2 + comp]])
            tt("dve", o1, bank(4), rden[0], ALU.mult, r=[PB[4], er[2]], w=[er[4]])
            tt("dve", o2, bank(5), rden[1], ALU.mult, r=[PB[5], er[3]], w=[er[5]])
            stt(at, o2, neglam[:, 0:1], o1, ALU.mult, ALU.add, r=[er[4], er[5], small], w=[er[6]])
            act(sqb, at, AF.Square, r=[er[6]], w=[er[0]])
            mm(bank(2), ones_b, sqb, True, True, r=[er[0], consts], w=[PB[2]])
            act(lnb, bank(2), AF.Ln, r=[PB[2]], w=[er[7]], bias=eps_c[:, 0:1], scale=1.0 / 128)
            act(rsb, lnb, AF.Exp, r=[er[7]], w=[er[4]], scale=-0.5)
            stt(mergedT[:, 8 + h, qc0:qc0 + 512], at, gsub2[:, 0:1], rsb, ALU.mult, ALU.mult,
                r=[er[6], er[4], small], w=[mg_r])
    if stage == 0.4:
        return early([(out_d[0:128, :], g1_bc, [small])])
    S.barrier()
    AR.release(mMG)

    mD = AR.mark()
    htb = HTB(1)
    hTd = AR.alloc([16, 512], BF16)
    hTd_r = Res()
    wblk = [AR.alloc([16, 512], BF16) for _ in range(2)]
    wb_r = [Res(), Res()]
    gu = AR.alloc([4, 1024], BF16)
    gv = AR.alloc([4, 1024], F32)
    guv_r = [Res() for _ in range(4)]
    lng = AR.alloc([1024], F32)
    wsT_f = AR.alloc([8, 128], F32)
    wsT_b = AR.alloc([8, 128], BF16)
    bsT = AR.alloc([8], F32)
    tmpA = AR.alloc([1024], F32)
    tmpB = AR.alloc([1024], F32)
    vn = AR.alloc([1024], BF16)
    aout = AR.alloc([1024], BF16)
    st8 = AR.alloc([64], F32)
    dr = [Res() for _ in range(8)]
    gm = Res()
    dma("sp", lng, lng_d, w=[gm])
    dma("sp", wsT_f.rearrange("p a b -> p (a b)"), wsT_d, w=[gm])
    dma("sp", bsT, bsT_d, w=[gm])
    cp("dve", wsT_b, wsT_f, r=[gm], w=[gm])
    wctr = 0
    for g in range(NGO):
        for ti in range(4):
            htb.tile(x_own[g * 512 + ti * 128:g * 512 + (ti + 1) * 128, :], A1, B1, hTd, hTd_r, ti * 128, 0)
        for cb in range(4):
            wb, wbr = wblk[wctr % 2], wb_r[wctr % 2]
            wctr += 1
            dma("pool", wb, w_in[:, cb * 512:(cb + 1) * 512].rearrange("(k p) c -> p k c", p=128), w=[wbr])
            for ti in range(4):
                pb = 4 + ti
                for k in range(KD):
                    mm(bank(pb), hTd[:, k, ti * 128:(ti + 1) * 128], wb[:, k, :], k == 0, k == KD - 1,
                       r=[hTd_r, wbr], w=[PB[pb]])
                if cb < 2:
                    act(gu[:, ti, cb * 512:(cb + 1) * 512], bank(pb), AF.Gelu, r=[PB[pb]], w=[guv_r[ti]])
                else:
                    act(gv[:, ti, (cb - 2) * 512:(cb - 1) * 512], bank(pb), AF.Gelu, r=[PB[pb]], w=[guv_r[ti]])
        for ti in range(4):
            gv2 = gv[:, ti, :]
            gv3 = gv2.rearrange("p (a b) -> p a b", b=128)
            tA3 = tmpA.rearrange("p (a b) -> p a b", b=128)
            tB3 = tmpB.rearrange("p (a b) -> p a b", b=128)
            s1, s2, mean, msq = st8[:, 0:8], st8[:, 8:16], st8[:, 16:24], st8[:, 24:32]
            var, lnv, rstd, nmr = st8[:, 32:40], st8[:, 40:48], st8[:, 48:56], st8[:, 56:64]
            S.op("dve", lambda e, a=s1, b=gv3: e.tensor_reduce(a, b, AX.X, ALU.add), r=[guv_r[ti]], w=[dr[0]])
            act(tmpA, gv2, AF.Square, r=[guv_r[ti]], w=[dr[1]])
            S.op("dve", lambda e, a=s2, b=tA3: e.tensor_reduce(a, b, AX.X, ALU.add), r=[dr[1]], w=[dr[0]])
            ts("dve", mean, s1, 1.0 / 128, None, ALU.mult, None, r=[dr[0]], w=[dr[0]])
            tt("dve", msq, mean, mean, ALU.mult, r=[dr[0]], w=[dr[0]])
            stt(var, s2, 1.0 / 128, msq, ALU.mult, ALU.subtract, r=[dr[0]], w=[dr[0]])
            act(lnv, var, AF.Ln, r=[dr[0]], w=[dr[0]], bias=eps_c[:, 0:1])
            act(rstd, lnv, AF.Exp, r=[dr[0]], w=[dr[0]], scale=-0.5)
            stt(nmr, mean, -1.0, rstd, ALU.mult, ALU.mult, r=[dr[0]], w=[dr[0]])
            tt("dve", tA3, gv3, rstd.unsqueeze(2).to_broadcast([128, 8, 128]), ALU.mult,
               r=[guv_r[ti], dr[0], dr[1]], w=[dr[1]])
            tt("pool", tB3, tA3, nmr.unsqueeze(2).to_broadcast([128, 8, 128]), ALU.add,
               r=[dr[1], dr[0]], w=[dr[2]])
            tt("dve", vn, tmpB, lng, ALU.mult, r=[dr[2], gm], w=[dr[3]])
            for gi in range(8):
                pb = 2 + gi // 4
                mm(bank(pb, 128, (gi % 4) * 128), wsT_b[:, gi, :], vn[:, gi * 128:(gi + 1) * 128], True, True,
                   r=[gm, dr[3]], w=[PB[pb]])
            for gi in range(8):
                pb = 2 + gi // 4
                stt(aout[:, gi * 128:(gi + 1) * 128], bank(pb, 128, (gi % 4) * 128), bsT[:, gi:gi + 1],
                    gu[:, ti, gi * 128:(gi + 1) * 128], ALU.add, ALU.mult, r=[PB[pb], gm, guv_r[ti]], w=[dr[4]])
            pTb = bank_bf(0, 1)
            for gi in range(8):
                tr(pTb[:, gi * 128:(gi + 1) * 128], aout[:, gi * 128:(gi + 1) * 128], ident_b,
                   r=[dr[4], consts], w=[PB[0]])
            tok0 = g * 512 + ti * 128
            cp("act", mergedT[:, 0:8, tok0:tok0 + 128], pTb.rearrange("p (a b) -> p a b", b=128),
               r=[PB[0]], w=[mg_r])
    if stage == 0.5:
        return early([(out_d[0:128, :], g1_bc, [small])])
    S.barrier()
    AR.release(mD)

    mE = AR.mark()
    wo = AR.alloc([16, D], BF16)
    wo_r = Res()
    for k in range(KD):
        dma("pool", wo[:, k, :], w_out[k * 128:(k + 1) * 128, :], w=[wo_r])
    xt2 = [AR.alloc([D], F32) for _ in range(2)]
    xt2_r = [Res(), Res()]
    xnb = [AR.alloc([D], F32) for _ in range(2)]
    xnb_r = [Res(), Res()]
    tmpE = AR.alloc([D], F32)
    tmpE_r = Res()
    out_ops = []
    for ti in range(NTO):
        xt, xr = xt2[ti % 2], xt2_r[ti % 2]
        xo, xor_ = xnb[ti % 2], xnb_r[ti % 2]
        dma("sp", xt, x_own[ti * 128:(ti + 1) * 128, :], w=[xr])
        b0 = 4 * (ti % 2)
        for db in range(4):
            for c in range(16):
                mm(bank(b0 + db), mergedT[:, c, ti * 128:(ti + 1) * 128], wo[:, c, db * 512:(db + 1) * 512],
                   c == 0, c == 15, r=[mg_r, wo_r], w=[PB[b0 + db]])
            tt("dve", tmpE[:, db * 512:(db + 1) * 512], bank(b0 + db), g1_bc[:, db * 512:(db + 1) * 512], ALU.mult,
               r=[PB[b0 + db], small], w=[tmpE_r])
        tt("pool", xo, tmpE, xt, ALU.add, r=[tmpE_r, xr], w=[xor_])
        if stage == 1:
            out_ops.append(dma("pool", out_d[ti * 128:(ti + 1) * 128, :], xo, r=[xor_]))
        else:
            out_ops.append(dma("pool", xn_scr[ti * 128:(ti + 1) * 128, :], xo, r=[xor_]))
    if stage == 1:
        S.finish(out_ops)
        S.emit(nc)
        return nc
    S.barrier()
    AR.release(persist_mark)

    h2T = AR.alloc([16, 512], BF16)
    h2_r = Res()
    htb = HTB(1)
    scs = AR.alloc([4, 16, 128], F32)
    scs_r = [Res() for _ in range(4)]
    thr = AR.alloc([4, 8], F32)
    nbt = AR.alloc([4, 8], F32)
    tn_r = [Res() for _ in range(4)]
    yacc = AR.alloc([4, D], F32)
    y_r = [Res() for _ in range(4)]
    keysb = AR.alloc([16, 128], BF16)
    keys_r = Res()
    dma("pool", keysb.rearrange("p a b -> p (a b)"), keysT_d, w=[keys_r])
    mF = AR.mark()
    fin_ops = []
    NEB = NEXP // 512
    for blk in range(NGO):
        S.barrier()
        AR.release(mF)
        for ti in range(4):
            r0 = blk * 512 + ti * 128
            htb.tile(xn_scr[r0:r0 + 128, :], A2, B2, h2T, h2_r, ti * 128, 0)
        qTs = AR.alloc([16, 512], BF16)
        q_r = Res()
        wqc = [AR.alloc([16, 128], BF16) for _ in range(2)]
        wqc_r = [Res(), Res()]
        for hc in range(16):
            wq_, wqr = wqc[hc % 2], wqc_r[hc % 2]
            dma("pool", wq_, wq_d[:, hc * 128:(hc + 1) * 128].rearrange("(k p) c -> p k c", p=128), w=[wqr])
            pb = 2 + hc % 2
            for k in range(KD):
                mm(bank(pb), wq_[:, k, :], h2T[:, k, :], k == 0, k == KD - 1, r=[wqr, h2_r], w=[PB[pb]])
            cp("act", qTs[:, hc, :], bank(pb), r=[PB[pb]], w=[q_r])
        for ti in range(4):
            for q4 in range(4):
                pb = 4 + q4 % 2
                for j in range(4):
                    hc = q4 * 4 + j
                    mm(bank(pb, 128, j * 128), qTs[:, hc, ti * 128:(ti + 1) * 128], keysb[:, hc, :], True, True,
                       r=[q_r, keys_r], w=[PB[pb]])
                cp("act", scs[:, ti, q4 * 4:(q4 + 1) * 4, :], bank(pb).rearrange("p (a b) -> p a b", b=128),
                   r=[PB[pb]], w=[scs_r[ti]])
        t16 = AR.alloc([16, 16], F32)
        work = AR.alloc([128], F32)
        cand = AR.alloc([8, 256], F32)
        c16 = AR.alloc([8, 16], F32)
        work2 = AR.alloc([256], F32)
        dd = AR.alloc([8, 16], F32)
        zz = AR.alloc([16], F32)
        tk = Res()
        for ti in range(4):
            for hc in range(16):
                S.op("dve", lambda e, a=t16[:, hc, 0:8], b=scs[:, ti, hc, :]: e.max(a, b), r=[scs_r[ti]], w=[tk])
                S.op("dve", lambda e, a=work, b=t16[:, hc, 0:8], c=scs[:, ti, hc, :]: e.match_replace(a, b, c, -1e30),
                     r=[scs_r[ti], tk], w=[tk])
                S.op("dve", lambda e, a=t16[:, hc, 8:16], b=work: e.max(a, b), r=[tk], w=[tk])
            t16v = t16.rearrange("p (h c) k -> p h c k", c=2)
            ta = t16v[:, :, 0, :].unsqueeze(3).to_broadcast([128, 8, 16, 16])
            tb = t16v[:, :, 1, :].unsqueeze(2).to_broadcast([128, 8, 16, 16])
            tt("dve", cand.rearrange("p h (a b) -> p h a b", b=16), ta, tb, ALU.add, r=[tk], w=[tk])
            for h in range(8):
                S.op("dve", lambda e, a=c16[:, h, 0:8], b=cand[:, h, :]: e.max(a, b), r=[tk], w=[tk])
                S.op("dve", lambda e, a=work2, b=c16[:, h, 0:8], c=cand[:, h, :]: e.match_replace(a, b, c, -1e30),
                     r=[tk], w=[tk])
                S.op("dve", lambda e, a=c16[:, h, 8:16], b=work2: e.max(a, b), r=[tk], w=[tk])
            tt("dve", dd, c16, c16[:, :, 0:1].to_broadcast([128, 8, 16]), ALU.subtract, r=[tk], w=[tk])
            act(dd, dd, AF.Exp, r=[tk], w=[tk])
            S.op("dve", lambda e, a=zz[:, 0:8], b=dd: e.tensor_reduce(a, b, AX.X, ALU.add), r=[tk], w=[tk])
            act(zz[:, 8:16], zz[:, 0:8], AF.Ln, r=[tk], w=[tk])
            stt(nbt[:, ti, :], c16[:, :, 0], -1.0, zz[:, 8:16], ALU.mult, ALU.subtract, r=[tk], w=[tn_r[ti]])
            ts("dve", thr[:, ti, :], c16[:, :, 15], -2e-5, None, ALU.add, None, r=[tk], w=[tn_r[ti]])
        S.barrier()
        AR.release(mF)
        Ub = AR.alloc([4, D], BF16)
        Ub_r = Res()
        Vb = [AR.alloc([4, D], BF16)] * 2
        Vb_r = [Res()] * 2
        UT = AR.alloc([16, 512], BF16)
        UT_r = Res()
        gaT = [AR.alloc([512], BF16) for _ in range(4)]
        ga_r = [Res() for _ in range(4)]
        NSB = 4
        Eb = [AR.alloc([512], F32) for _ in range(NSB)]
        Eb_r = [[Res() for _ in range(4)] for _ in range(NSB)]
        NGH = 4
        Gh = [AR.alloc([512], BF16) for _ in range(NGH)]
        Gh_r = [[Res() for _ in range(4)] for _ in range(NGH)]
        bias2e = [AR.alloc([4, 8, 4], F32) for _ in range(2)]
        thr2e = [AR.alloc([4, 8, 4], F32) for _ in range(2)]
        b2_r = [Res(), Res()]
        PT = [AR.alloc([512], BF16) for _ in range(4)]
        PT_r = [Res() for _ in range(4)]
        sctr = 0
        gctr = 0
        yb = 0
        ectr = 0
        for eb in range(NEB):
            vb, vbr = Vb[eb % 2], Vb_r[eb % 2]
            dma("pool", Ub, U_d[eb * 512:(eb + 1) * 512, :].rearrange("(c p) d -> p c d", p=128), w=[Ub_r])
            dma("pool", vb, V_d[eb * 512:(eb + 1) * 512, :].rearrange("(c p) d -> p c d", p=128), w=[vbr])
            pTb = bank_bf(0, 1)
            for ec in range(4):
                for kg in range(2):
                    for kk in range(8):
                        k = kg * 8 + kk
                        tr(pTb[:, kk * 128:(kk + 1) * 128], Ub[:, ec, k * 128:(k + 1) * 128], ident_b,
                           r=[Ub_r, consts], w=[PB[0]])
                    eng = "act" if ectr % 2 == 0 else "dve"
                    ectr += 1
                    cp(eng, UT[:, kg * 8:(kg + 1) * 8, ec * 128:(ec + 1) * 128],
                       pTb.rearrange("p (a b) -> p a b", b=128), r=[PB[0]], w=[UT_r])
            for ec in range(4):
                for k in range(KD):
                    mm(bank(1), UT[:, k, ec * 128:(ec + 1) * 128], h2T[:, k, :], k == 0, k == KD - 1,
                       r=[UT_r, h2_r], w=[PB[1]])
                act(gaT[ec], bank(1), AF.Gelu, r=[PB[1]], w=[ga_r[ec]])
            b2, t2e, b2r = bias2e[eb % 2], thr2e[eb % 2], b2_r[eb % 2]
            sav = scs.rearrange("p t (h c) n -> p t h c n", c=2)[:, :, :, 0, eb * 4:(eb + 1) * 4]
            tt("dve", b2, sav, nbt.unsqueeze(3).to_broadcast([128, 4, 8, 4]), ALU.add,
               r=scs_r + tn_r, w=[b2r])
            tt("dve", t2e, thr.unsqueeze(3).to_broadcast([128, 4, 8, 4]), sav, ALU.subtract,
               r=scs_r + tn_r, w=[b2r])
            for ti in range(4):
                for h in range(8):
                    eb_, ebr = Eb[sctr % NSB], Eb_r[sctr % NSB]
                    sctr += 1
                    gh, ghr = Gh[gctr % NGH], Gh_r[gctr % NGH]
                    gctr += 1
                    sbv = scs[:, ti, 2 * h + 1, :]
                    for ii in range(4):
                        act(eb_[:, ii * 128:(ii + 1) * 128], sbv, AF.Exp, r=[scs_r[ti], b2r], w=[ebr[ii]],
                            bias=b2[:, ti, h, ii:ii + 1])
                    for ii in range(4):
                        stt(gh[:, ii * 128:(ii + 1) * 128], sbv, t2e[:, ti, h, ii:ii + 1],
                            eb_[:, ii * 128:(ii + 1) * 128], ALU.is_ge, ALU.mult,
                            r=[scs_r[ti], ebr[ii], b2r], w=[ghr[ii]])
                    for ec in range(4):
                        mm(bank(4 + ec, 128, ti * 128), gh[:, ec * 128:(ec + 1) * 128], ident_b, h == 0, h == 7,
                           r=[ghr[ec], consts], w=[PB[4 + ec]])
            for ec in range(4):
                tt("dve", PT[ec], bank(4 + ec), gaT[ec], ALU.mult, r=[PB[4 + ec], ga_r[ec]], w=[PT_r[ec]])
            for ti in range(4):
                for db in range(4):
                    pb = 2 + yb % 2
                    yb += 1
                    for ec in range(4):
                        mm(bank(pb), PT[ec][:, ti * 128:(ti + 1) * 128], vb[:, ec, db * 512:(db + 1) * 512],
                           ec == 0, ec == 3, r=[PT_r[ec], vbr], w=[PB[pb]])
                    ysl = yacc[:, ti, db * 512:(db + 1) * 512]
                    if eb == 0:
                        cp("dve", ysl, bank(pb), r=[PB[pb]], w=[y_r[ti]])
                    else:
                        tt("dve", ysl, bank(pb), ysl, ALU.add, r=[PB[pb], y_r[ti]], w=[y_r[ti]])
        for ti in range(4):
            r0 = blk * 512 + ti * 128
            xt, xr = htb.xt[0], htb.xt_r[0]
            dma("sp", xt, xn_scr[r0:r0 + 128, :], w=[xr])
            tt("dve", yacc[:, ti, :], yacc[:, ti, :], g2_bc, ALU.mult, r=[y_r[ti], small], w=[y_r[ti]])
            tt("pool", yacc[:, ti, :], yacc[:, ti, :], xt, ALU.add, r=[y_r[ti], xr], w=[y_r[ti]])
            fin_ops.append(dma("pool", out_d[r0:r0 + 128, :], yacc[:, ti, :], r=[y_r[ti]]))
    S.finish(fin_ops)
    S.emit(nc)
    return nc


_PROG = {}


def _rope_tables(SEQ):
    n_rows = SEQ // 64
    rows = np.repeat(np.arange(n_rows, dtype=np.float32), 64)
    cols = np.tile(np.arange(64, dtype=np.float32), n_rows)
    inv = (np.float32(10000.0) ** (-np.arange(16, dtype=np.float32) / np.float32(16))).astype(np.float32)
    ang = np.concatenate([rows[:, None] * inv, cols[:, None] * inv], axis=-1).astype(np.float32)
    cos = np.cos(ang).astype(np.float32)
    sin = np.sin(ang).astype(np.float32)
    p = np.arange(128)
    fi = (p % 64) // 2
    sgn = np.where((p % 2) == 0, -1.0, 1.0).astype(np.float32)
    cosT = np.ascontiguousarray(cos[:, fi].T)
    sinT = np.ascontiguousarray((sin[:, fi] * sgn[None, :]).T)
    return cosT, sinT


def kernel(x, c, ctx, c_ctx, w_ada, b_ada, norm1_g, norm2_g, w_in, gmlp_ln_g, gmlp_ws, gmlp_bs,
           q_norm_g, k_norm_g, lambda_q1, lambda_k1, lambda_q2, lambda_k2, subln_g, w_out,
           peer_wq, peer_keys, peer_u, peer_v, _stage=2, _cores=NCORES):
    f = lambda a: np.ascontiguousarray(np.asarray(a, dtype=np.float32))
    x = f(x)
    SEQ = x.shape[1]
    TPC = SEQ // NCORES
    key = (SEQ, _stage)
    if key not in _PROG:
        _PROG[key] = build_program(SEQ, _stage)
    nc = _PROG[key]
    x2 = x[0]
    cc = np.stack([f(c)[0], f(c_ctx)], axis=-1)
    cT = np.ascontiguousarray(cc.reshape(16, 128, 2).transpose(1, 0, 2).reshape(128, 32))
    b = f(b_ada)[0]
    bT = np.ascontiguousarray(b.reshape(96, 128).T)
    b_gate = np.ascontiguousarray(np.tile(np.concatenate([b[2 * D:3 * D], b[5 * D:6 * D]])[None, :], (128, 1)))
    n1g = np.ascontiguousarray(f(norm1_g)[0].reshape(16, 128).T)
    n2g = np.ascontiguousarray(f(norm2_g)[0].reshape(16, 128).T)
    lng_bc = np.ascontiguousarray(np.tile(f(gmlp_ln_g)[0][None, :], (128, 1)))
    wsT = np.ascontiguousarray(f(gmlp_ws)[0].transpose(2, 0, 1).reshape(128, 8 * 128))
    bsT = np.ascontiguousarray(f(gmlp_bs)[0].T)
    _qg, _kg = np.tile(f(q_norm_g)[0], 2), np.tile(f(k_norm_g)[0], 2)
    qkg = np.ascontiguousarray(np.stack([_qg, _qg, _kg, _kg], axis=-1))
    lam_a = np.ascontiguousarray(np.concatenate([f(lambda_q1)[0], f(lambda_q2)[0]])[None, :])
    lam_b = np.ascontiguousarray(np.concatenate([f(lambda_k1)[0], f(lambda_k2)[0]])[None, :])
    gsub = np.ascontiguousarray(f(subln_g)[0][:, None])
    keysT = np.ascontiguousarray(f(peer_keys)[0].transpose(3, 0, 1, 2).reshape(128, 16 * 128))
    cosT, sinT = _rope_tables(SEQ)
    ident = np.eye(128, dtype=np.float32)
    idx = np.arange(128)
    pswap = np.zeros((128, 128), np.float32)
    pswap[idx ^ 1, idx] = 1.0
    blockones = (idx[:, None] // 64 == idx[None, :] // 64).astype(np.float32)
    common = {
        "x_all": x2, "ctx": f(ctx)[0], "cT": cT, "w_ada": f(w_ada)[0], "bT": bT, "b_gate": b_gate,
        "n1g": n1g, "n2g": n2g, "w_in": f(w_in)[0], "lng_bc": lng_bc, "wsT": wsT, "bsT": bsT, "qkg": qkg,
        "lam_a": lam_a, "lam_b": lam_b, "gsub": gsub, "w_out": f(w_out)[0], "wq": f(peer_wq)[0],
        "keysT": keysT, "peer_u": f(peer_u)[0], "peer_v": f(peer_v)[0], "cosT": cosT, "sinT": sinT,
        "ident": ident, "pswap": pswap, "blockones": blockones,
    }
    in_maps = []
    for r in range(NCORES):
        m = dict(common)
        m["x_own"] = np.ascontiguousarray(x2[r * TPC:(r + 1) * TPC])
        m["cos_own"] = np.ascontiguousarray(cosT[:, r * TPC:(r + 1) * TPC])
        m["sin_own"] = np.ascontiguousarray(sinT[:, r * TPC:(r + 1) * TPC])
        in_maps.append(m)
    if _stage < 2:
        for m in in_maps:
            for kk in ("wq", "keysT", "peer_u", "peer_v"):
                m.pop(kk, None)
            if _stage in (0.15, 0.16, 0.17):
                m.pop("w_ada", None)
    in_maps = in_maps[:_cores]
    res = run_bass_kernel_spmd(nc, in_maps, core_ids=list(range(_cores)))
    out = np.concatenate([np.asarray(res.results[r]["out"], dtype=np.float32) for r in range(_cores)], axis=0)
    return out[None, :, :]
```
